# Optimizing a Trainium2 kernel written in Bass

```python
import math
import jax, jax.numpy as jnp
from jax import lax
import numpy as np

D_MODEL = 1024
BATCH = 4
SEQ = 8192
DEPTH = 2

N_MEM = 256
GROUP_WIDTH = 512
N_GROUPS = 3
MIX_WIDTH = N_GROUPS * GROUP_WIDTH
ATT_HEADS = 8
ATT_HEAD_DIM = GROUP_WIDTH // ATT_HEADS
MOBA_BLOCK = 256
MOBA_TOPK = 3
Q_CHUNK = 128
LRU_HEADS = 8
LRU_HEAD_DIM = GROUP_WIDTH // LRU_HEADS
LRU_CONV = 4
LRU_C = 8.0
SC_CONV = 3
XATTN_HEADS = 4
XATTN_HEAD_DIM = D_MODEL // XATTN_HEADS
N_IN_SPLITS = 10
IN_COLS = N_IN_SPLITS * GROUP_WIDTH
DN_ALPHA = (2.0 * DEPTH) ** 0.25
DN_BETA = (8.0 * DEPTH) ** -0.25
LN_EPS = 1e-5
RMS_EPS = 1e-6

kernel_name = 'hymba_moba_rglru_shortconv_deepnorm'


def layer_norm(x, g, b):
    xf = x.astype(jnp.float32)
    mu = jnp.mean(xf, axis=-1, keepdims=True)
    var = jnp.mean(jnp.square(xf - mu), axis=-1, keepdims=True)
    return ((xf - mu) * lax.rsqrt(var + LN_EPS) * g.astype(jnp.float32) + b.astype(jnp.float32)).astype(x.dtype)


def group_rms_norm(y, g):
    yf = y.astype(jnp.float32)
    r = lax.rsqrt(jnp.mean(yf * yf, axis=-1, keepdims=True) + RMS_EPS)
    return (yf * r * g.astype(jnp.float32)).astype(y.dtype)


def causal_dwconv(x, w):
    k, c = w.shape
    return lax.conv_general_dilated(
        x, w[:, None, :].astype(x.dtype), window_strides=(1,), padding=[(k - 1, 0)],
        dimension_numbers=('NWC', 'WIO', 'NWC'), feature_group_count=c)


def moba_attention(q, k, v):
    b, h, s, dh = q.shape
    n_blk = -(-s // MOBA_BLOCK)
    s_pad = n_blk * MOBA_BLOCK
    pad = ((0, 0), (0, 0), (0, s_pad - s), (0, 0))
    k_blocks = jnp.pad(k, pad).reshape(b, h, n_blk, MOBA_BLOCK, dh)
    v_blocks = jnp.pad(v, pad).reshape(b, h, n_blk, MOBA_BLOCK, dh)
    k_mean = jnp.mean(k_blocks.astype(jnp.float32), axis=3).astype(k.dtype)
    n_sel = min(MOBA_TOPK, n_blk)
    scale = dh ** -0.5
    blk_ids = jnp.arange(n_blk)
    gather = jax.vmap(jax.vmap(lambda blocks, idx: blocks[idx]))

    def chunk(ci):
        start = ci * Q_CHUNK
        qc = lax.dynamic_slice_in_dim(q, start, Q_CHUNK, axis=2)
        own = start // MOBA_BLOCK
        q_pos = start + jnp.arange(Q_CHUNK)
        gate = jnp.einsum('bhqd,bhnd->bhqn', qc, k_mean).astype(jnp.float32)
        gate = jnp.where(blk_ids < own, gate, -jnp.inf)
        _, sel = lax.top_k(gate, n_sel)
        sel_valid = jnp.arange(n_sel) < own
        k_sel = gather(k_blocks, sel)
        v_sel = gather(v_blocks, sel)
        k_own = lax.dynamic_slice_in_dim(k_blocks, own, 1, axis=2)[:, :, 0]
        v_own = lax.dynamic_slice_in_dim(v_blocks, own, 1, axis=2)[:, :, 0]
        s_sel = jnp.einsum('bhqd,bhqnkd->bhqnk', qc, k_sel).astype(jnp.float32) * scale
        s_sel = jnp.where(sel_valid[:, None], s_sel, -jnp.inf)
        s_own = jnp.einsum('bhqd,bhkd->bhqk', qc, k_own).astype(jnp.float32) * scale
        k_pos = own * MOBA_BLOCK + jnp.arange(MOBA_BLOCK)
        s_own = jnp.where(k_pos[None, :] <= q_pos[:, None], s_own, -jnp.inf)
        n_sk = n_sel * MOBA_BLOCK
        scores = jnp.concatenate([s_sel.reshape(b, h, Q_CHUNK, n_sk), s_own], axis=-1)
        p = jax.nn.softmax(scores, axis=-1)
        p_sel = p[..., :n_sk].reshape(b, h, Q_CHUNK, n_sel, MOBA_BLOCK).astype(v.dtype)
        p_own = p[..., n_sk:].astype(v.dtype)
        return (jnp.einsum('bhqnk,bhqnkd->bhqd', p_sel, v_sel)
                + jnp.einsum('bhqk,bhkd->bhqd', p_own, v_own))

    outs = lax.map(chunk, jnp.arange(s // Q_CHUNK))
    return outs.transpose(1, 2, 0, 3, 4).reshape(b, h, s, dh)


def rg_lru(x, w_a, b_a, w_x, b_x, lam):
    b, s, w = x.shape
    xh = x.reshape(b, s, LRU_HEADS, LRU_HEAD_DIM)
    r = jax.nn.sigmoid(jnp.einsum('bshi,hij->bshj', xh, w_a).reshape(b, s, w) + b_a)
    i = jax.nn.sigmoid(jnp.einsum('bshi,hij->bshj', xh, w_x).reshape(b, s, w) + b_x)
    log_a = -LRU_C * r.astype(jnp.float32) * jax.nn.softplus(-lam.astype(jnp.float32))
    a = jnp.exp(log_a)
    u = jnp.sqrt(-jnp.expm1(2.0 * log_a)) * (i * x).astype(jnp.float32)

    def combine(c1, c2):
        a1, b1 = c1
        a2, b2 = c2
        return a1 * a2, a2 * b1 + b2

    _, hs = lax.associative_scan(combine, (a, u), axis=1)
    return hs.astype(x.dtype)


def hybrid_mixer(x, w_in, lru_conv_w, lru_conv_b, lru_w_a, lru_b_a, lru_w_x, lru_b_x,
                 lru_lambda, sc_conv_w, group_gain, w_out):
    b, s, _ = x.shape
    proj = x @ w_in
    q, k, v, g_att, x_lru, g_lru, sc_b, sc_c, sc_x, g_sc = jnp.split(proj, N_IN_SPLITS, axis=-1)
    heads = lambda t: t.reshape(b, s, ATT_HEADS, ATT_HEAD_DIM).transpose(0, 2, 1, 3)
    y_att = moba_attention(heads(q), heads(k), heads(v)).transpose(0, 2, 1, 3).reshape(b, s, GROUP_WIDTH)
    xl = causal_dwconv(x_lru, lru_conv_w) + lru_conv_b
    y_lru = rg_lru(xl, lru_w_a, lru_b_a, lru_w_x, lru_b_x, lru_lambda)
    y_sc = sc_b * causal_dwconv(sc_c * sc_x, sc_conv_w)
    y = jnp.stack([y_att, y_lru, y_sc], axis=2)
    gates = jnp.stack([g_att, g_lru, g_sc], axis=2)
    y = group_rms_norm(y, group_gain) * jax.nn.silu(gates)
    return y.reshape(b, s, MIX_WIDTH) @ w_out


def memory_cross_attention(x, mem, wq, wk, wv, wo):
    b, s, _ = x.shape
    m = mem.shape[1]
    q = (x @ wq).reshape(b, s, XATTN_HEADS, XATTN_HEAD_DIM)
    k = (mem @ wk).reshape(b, m, XATTN_HEADS, XATTN_HEAD_DIM)
    v = (mem @ wv).reshape(b, m, XATTN_HEADS, XATTN_HEAD_DIM)
    scores = jnp.einsum('bshd,bmhd->bhsm', q, k).astype(jnp.float32) * (XATTN_HEAD_DIM ** -0.5)
    p = jax.nn.softmax(scores, axis=-1).astype(v.dtype)
    o = jnp.einsum('bhsm,bmhd->bshd', p, v).reshape(b, s, D_MODEL)
    return o @ wo


def setup_inputs(seed: int = 0) -> dict:
    key = jax.random.key(seed)
    ks = jax.random.split(key, 24)
    f32 = jnp.float32
    nrm = lambda k, shape, scale: jax.random.normal(k, shape, f32) * scale
    L = DEPTH
    G = GROUP_WIDTH
    u = jax.random.uniform(ks[9], (L, G), f32, 0.9, 0.999)
    p = u ** (1.0 / LRU_C)
    return {
        'x': nrm(ks[0], (BATCH, SEQ, D_MODEL), 1.0),
        'mem': nrm(ks[1], (BATCH, N_MEM, D_MODEL), 1.0),
        'w_in': nrm(ks[2], (L, D_MODEL, IN_COLS), D_MODEL ** -0.5),
        'lru_conv_w': nrm(ks[3], (L, LRU_CONV, G), LRU_CONV ** -0.5),
        'lru_conv_b': nrm(ks[4], (L, G), 0.02),
        'lru_w_a': nrm(ks[5], (L, LRU_HEADS, LRU_HEAD_DIM, LRU_HEAD_DIM), LRU_HEAD_DIM ** -0.5),
        'lru_b_a': nrm(ks[6], (L, G), 0.02),
        'lru_w_x': nrm(ks[7], (L, LRU_HEADS, LRU_HEAD_DIM, LRU_HEAD_DIM), LRU_HEAD_DIM ** -0.5),
        'lru_b_x': nrm(ks[8], (L, G), 0.02),
        'lru_lambda': jnp.log(p) - jnp.log1p(-p),
        'sc_conv_w': nrm(ks[10], (L, SC_CONV, G), SC_CONV ** -0.5),
        'group_gain': 1.0 + nrm(ks[11], (L, N_GROUPS, G), 0.02),
        'w_out': nrm(ks[12], (L, MIX_WIDTH, D_MODEL), MIX_WIDTH ** -0.5 * DN_BETA),
        'ln1_g': 1.0 + nrm(ks[13], (L, D_MODEL), 0.02),
        'ln1_b': nrm(ks[14], (L, D_MODEL), 0.02),
        'xq_w': nrm(ks[15], (L, D_MODEL, D_MODEL), D_MODEL ** -0.5),
        'xk_w': nrm(ks[16], (L, D_MODEL, D_MODEL), D_MODEL ** -0.5),
        'xv_w': nrm(ks[17], (L, D_MODEL, D_MODEL), D_MODEL ** -0.5),
        'xo_w': nrm(ks[18], (L, D_MODEL, D_MODEL), D_MODEL ** -0.5 * DN_BETA),
        'ln2_g': 1.0 + nrm(ks[19], (L, D_MODEL), 0.02),
        'ln2_b': nrm(ks[20], (L, D_MODEL), 0.02),
    }


def reference(x, mem, w_in, lru_conv_w, lru_conv_b, lru_w_a, lru_b_a, lru_w_x, lru_b_x,
              lru_lambda, sc_conv_w, group_gain, w_out, ln1_g, ln1_b,
              xq_w, xk_w, xv_w, xo_w, ln2_g, ln2_b):
    for l in range(DEPTH):
        mix = hybrid_mixer(x, w_in[l], lru_conv_w[l], lru_conv_b[l], lru_w_a[l], lru_b_a[l],
                           lru_w_x[l], lru_b_x[l], lru_lambda[l], sc_conv_w[l], group_gain[l], w_out[l])
        x = layer_norm(DN_ALPHA * x + mix, ln1_g[l], ln1_b[l])
        xat = memory_cross_attention(x, mem, xq_w[l], xk_w[l], xv_w[l], xo_w[l])
        x = layer_norm(DN_ALPHA * x + xat, ln2_g[l], ln2_b[l])
    return x
```

```python
from contextlib import ExitStack
import numpy as np
import concourse.bass as bass
import concourse.mybir as mybir
from concourse.bass_utils import run_bass_kernel_spmd

F32 = mybir.dt.float32
BF16 = mybir.dt.bfloat16
ALU = mybir.AluOpType
AF = mybir.ActivationFunctionType
AX = mybir.AxisListType

S = 8192
D = 1024
L = 2
NMEM = 256
NT = S // 512
ALPHA = float((2.0 * L) ** 0.25)
BIG = 1000.0
NPP = 52
STOP = None
NHEAD_RUN = 8
LN_EPS = 1e-5
RMS_EPS = 1e-6

ENGS = ['pe', 'act', 'dve', 'pool', 'sp']


class V:
    __slots__ = ('ap', 'key')

    def __init__(self, ap, key):
        self.ap = ap
        self.key = key


class _Slot:
    def __init__(self, t, slot):
        self.t = t
        self.slot = slot

    def __getitem__(self, idx):
        return V(self.t.ap[idx], (self.t.name, self.slot))


class TT:
    def __init__(self, name, ap):
        self.name = name
        self.ap = ap

    def __getitem__(self, idx):
        return V(self.ap[idx], (self.name, None))

    def at(self, slot):
        return _Slot(self, slot)


class Prog:
    def __init__(self, nc, es, n_dma_sems=32):
        self.nc = nc
        self.q = {e: [] for e in ENGS}
        self.sem = {e: es.enter_context(nc.semaphore('s_' + e)) for e in ENGS}
        self.cnt = {e: 0 for e in ENGS}
        self.dsem = [es.enter_context(nc.semaphore('d%d' % i)) for i in range(n_dma_sems)]
        self.dcnt = [0] * n_dma_sems
        self.drr = 0
        self.waited = {e: {} for e in ENGS}
        self.state = {}
        self.sems_by_id = {}
        self.n_wait = 0
        self.n_inst = 0
        for e in ENGS:
            self._semid(self.sem[e])
        for s in self.dsem:
            self._semid(s)

    def _semid(self, s):
        i = id(s)
        self.sems_by_id[i] = s
        return i

    def _slots(self, key):
        name, slot = key
        d = self.state.setdefault(name, {})
        if slot is None:
            if None not in d:
                d[None] = {'w': None, 'r': []}
            return list(d.values())
        if slot not in d:
            d[slot] = {'w': None, 'r': []}
        out = [d[slot]]
        if None in d:
            out.append(d[None])
        return out

    def _need(self, toks, reads, writes):
        for k in reads:
            for st in self._slots(k):
                if st['w'] is not None:
                    toks.append(st['w'])
        for k in writes:
            for st in self._slots(k):
                if st['w'] is not None:
                    toks.append(st['w'])
                toks.extend(st['r'])

    def _update(self, tok, reads, writes):
        for (name, slot) in reads:
            st = self.state[name][slot]
            st['r'] = [t for t in st['r'] if t[0] != tok[0]] + [tok]
        for (name, slot) in writes:
            d = self.state[name]
            if slot is None:
                for s in list(d.keys()):
                    d[s] = {'w': tok, 'r': []}
            else:
                d[slot] = {'w': tok, 'r': []}

    def _emit_waits(self, eng, toks):
        best = {}
        for (sid, v) in toks:
            if v > best.get(sid, 0):
                best[sid] = v
        w = self.waited[eng]
        for sid, v in best.items():
            if w.get(sid, 0) >= v:
                continue
            w[sid] = v
            s = self.sems_by_id[sid]
            self.q[eng].append(lambda e, s=s, v=v: e.wait_ge(s, v))
            self.n_wait += 1

    def op(self, eng, fn, reads=(), writes=(), inc=True):
        toks = []
        self._need(toks, reads, writes)
        sid = id(self.sem[eng])
        cur = self.cnt[eng] + 1
        toks = [t for t in toks if not (t[0] == sid and t[1] >= cur)]
        self._emit_waits(eng, toks)
        if inc:
            s = self.sem[eng]
            self.q[eng].append(lambda e, fn=fn, s=s: fn(e).then_inc(s, 1))
            self.cnt[eng] = cur
        else:
            self.q[eng].append(lambda e, fn=fn: fn(e))
        self._update((sid, cur), reads, writes)
        self.n_inst += 1

    def dma(self, eng, out, in_, **kw):
        reads = [in_.key] if in_.key is not None else []
        writes = [out.key] if out.key is not None else []
        toks = []
        self._need(toks, reads, writes)
        j = self.drr
        self.drr = (self.drr + 1) % len(self.dsem)
        s = self.dsem[j]
        sid = id(s)
        if self.dcnt[j] > 0:
            toks.append((sid, self.dcnt[j]))
        self._emit_waits(eng, toks)
        self.dcnt[j] += 16
        tok = (sid, self.dcnt[j])
        self.q[eng].append(lambda e, o=out.ap, i=in_.ap, s=s, kw=kw: e.dma_start(out=o, in_=i, **kw).then_inc(s, 16))
        self._update(tok, reads, writes)
        self.n_inst += 1
        return tok

    def barrier(self):
        toks = [(id(self.sem[e]), self.cnt[e]) for e in ENGS if self.cnt[e] > 0]
        toks += [(id(self.dsem[j]), self.dcnt[j]) for j in range(len(self.dsem)) if self.dcnt[j] > 0]
        for e in ENGS:
            own = id(self.sem[e])
            self._emit_waits(e, [t for t in toks if t[0] != own])

    def finish(self, final_toks):
        nc = self.nc
        self._emit_waits('sp', list(final_toks))
        q = self.q
        with nc.Block() as block:
            @block.tensor
            def _(e):
                for f in q['pe']:
                    f(e)

            @block.scalar
            def _(e):
                for f in q['act']:
                    f(e)

            @block.vector
            def _(e):
                for f in q['dve']:
                    f(e)

            @block.gpsimd
            def _(e):
                for f in q['pool']:
                    f(e)

            @block.sync
            def _(e):
                for f in q['sp']:
                    f(e)

    @staticmethod
    def _keys(*vs):
        out = []
        for v in vs:
            if isinstance(v, V) and v.key is not None:
                if isinstance(v.key, list):
                    out.extend(v.key)
                else:
                    out.append(v.key)
        return out

    @staticmethod
    def _a(v):
        return v.ap if isinstance(v, V) else v

    def mm(self, out, lhsT, rhs, start=True, stop=True, inc=True):
        self.op('pe', lambda e: e.matmul(out.ap, lhsT.ap, rhs.ap, start=start, stop=stop),
                reads=self._keys(lhsT, rhs), writes=self._keys(out), inc=inc)

    def tr(self, out, in_, ident, inc=True):
        self.op('pe', lambda e: e.transpose(out.ap, in_.ap, ident.ap),
                reads=self._keys(in_, ident), writes=self._keys(out), inc=inc)

    def act(self, out, in_, func, bias=None, scale=None, accum=None):
        kw = {}
        if bias is not None:
            kw['bias'] = self._a(bias)
        if scale is not None:
            kw['scale'] = self._a(scale)
        if accum is not None:
            kw['accum_out'] = accum.ap
        self.op('act', lambda e: e.activation(out.ap, in_.ap, func, **kw),
                reads=self._keys(in_, bias, scale), writes=self._keys(out, accum))

    def tt(self, eng, out, in0, in1, op):
        self.op(eng, lambda e: e.tensor_tensor(out.ap, in0.ap, in1.ap, op),
                reads=self._keys(in0, in1), writes=self._keys(out))

    def ts(self, eng, out, in0, s1, s2, op0, op1=None):
        a1, a2 = self._a(s1), self._a(s2)
        if op1 is None:
            self.op(eng, lambda e: e.tensor_scalar(out.ap, in0.ap, a1, None, op0),
                    reads=self._keys(in0, s1), writes=self._keys(out))
        else:
            self.op(eng, lambda e: e.tensor_scalar(out.ap, in0.ap, a1, a2, op0, op1),
                    reads=self._keys(in0, s1, s2), writes=self._keys(out))

    def stt(self, out, in0, scalar, in1, op0, op1):
        a = self._a(scalar)
        self.op('dve', lambda e: e.scalar_tensor_tensor(out.ap, in0.ap, a, in1.ap, op0, op1),
                reads=self._keys(in0, scalar, in1), writes=self._keys(out))

    def copy(self, eng, out, in_):
        if eng == 'act':
            self.op('act', lambda e: e.copy(out.ap, in_.ap), reads=self._keys(in_), writes=self._keys(out))
        else:
            self.op(eng, lambda e: e.tensor_copy(out.ap, in_.ap), reads=self._keys(in_), writes=self._keys(out))

    def memset(self, eng, out, val):
        self.op(eng, lambda e: e.memset(out.ap, val), writes=self._keys(out))

    def scan(self, out, d0, d1, init):
        a = self._a(init)
        self.op('dve', lambda e: e.tensor_tensor_scan(out.ap, d0.ap, d1.ap, a, ALU.mult, ALU.add),
                reads=self._keys(d0, d1, init), writes=self._keys(out))

    def max8(self, out, in_):
        self.op('dve', lambda e: e.max(out.ap, in_.ap), reads=self._keys(in_), writes=self._keys(out))

    def recip(self, out, in_):
        self.op('dve', lambda e: e.reciprocal(out.ap, in_.ap), reads=self._keys(in_), writes=self._keys(out))

    def reduce(self, out, in_, op, axis):
        self.op('dve', lambda e: e.tensor_reduce(out.ap, in_.ap, axis, op), reads=self._keys(in_), writes=self._keys(out))

    def bn_stats(self, out, in_):
        self.op('dve', lambda e: e.bn_stats(out.ap, in_.ap), reads=self._keys(in_), writes=self._keys(out))

    def bn_aggr(self, out, in_):
        self.op('dve', lambda e: e.bn_aggr(out.ap, in_.ap), reads=self._keys(in_), writes=self._keys(out))


class Arena:
    def __init__(self, nc, es, nwords):
        self.t = es.enter_context(nc.sbuf_tensor('arena', [128, nwords], F32))
        self.nwords = nwords
        self.off = 0
        self.uid = 0

    def reset(self):
        self.off = 0

    def alloc(self, name, shape, dt, parts=128):
        n = 1
        for s_ in shape:
            n *= s_
        nb = n * (2 if dt == BF16 else 4)
        nw = (nb + 3) // 4
        nw = (nw + 7) // 8 * 8
        assert self.off + nw <= self.nwords, ('arena overflow', name, self.off, nw, self.nwords)
        ap = self.t[0:parts, self.off:self.off + nw]
        self.off += nw
        if dt != F32:
            ap = ap.bitcast(dt)
        ap = ap[:, 0:n]
        if len(shape) == 2:
            ap = ap.rearrange('p (a b) -> p a b', a=shape[0])
        elif len(shape) == 3:
            ap = ap.rearrange('p (a b c) -> p a b c', a=shape[0], b=shape[1])
        self.uid += 1
        return TT('%s#%d' % (name, self.uid), ap)


class LoadCast:
    def __init__(self, P, stages, engs=('pool', 'dve', 'act')):
        self.P, self.stages, self.engs, self.i = P, stages, engs, 0

    def go(self, dst, src, n):
        st = self.stages[self.i % len(self.stages)]
        eng = self.engs[self.i % len(self.engs)]
        self.i += 1
        stv = V(st.ap[:, 0:n], st.key)
        self.P.dma('sp', stv, src)
        self.P.copy(eng, dst, stv)


def dram(ap, name, slot=None):
    return V(ap, (name, slot))


class Ctx:
    pass


def build_program(n_layers=L, stages='ABC', debug=False):
    nc = bass.Bass("TRN2", target_bir_lowering=False, dynamic_dma_scratch_size=8192)
    G = Ctx()
    ein = lambda n, shp: nc.dram_tensor(n, shp, F32, kind="ExternalInput").ap()
    G.x = ein("x", [S, D])
    G.memT = ein("memT", [D, NMEM])
    G.w_in = ein("w_in", [L, D, 5120])
    G.w_out = ein("w_out", [L, 1536, D])
    G.wq = ein("wq", [L, D, D])
    G.wk = ein("wk", [L, D, D])
    G.wv = ein("wv", [L, D, D])
    G.wo = ein("wo", [L, D, D])
    G.bd_a = ein("bd_a", [L, 4, 128, 128])
    G.bd_x = ein("bd_x", [L, 4, 128, 128])
    G.pp = ein("pp", [L, 128, NPP])
    G.bcp = ein("bcp", [L, 1, 512 + 4 * D])
    G.ident = ein("ident", [128, 128])
    G.E = ein("E", [32, S])
    G.trib = ein("trib", [2, 128, 256])
    G.vb = ein("vb", [2, 1, 64 * 32])
    G.out = nc.dram_tensor("out", [S, D], F32, kind="ExternalOutput").ap()
    kw = dict(kind="ExternalOutput") if debug else {}
    G.qT_d = nc.dram_tensor("qT_d", [512, S], BF16, **kw).ap()
    G.kT_d = nc.dram_tensor("kT_d", [512, S], BF16, **kw).ap()
    G.V_d = nc.dram_tensor("V_d", [S, 512], BF16, **kw).ap()
    G.gatt_d = nc.dram_tensor("gatt_d", [S, 512], F32, **kw).ap()
    G.zT_d = nc.dram_tensor("zT_d", [1024, S], BF16, **kw).ap()
    G.yatt_d = nc.dram_tensor("yatt_d", [S, 512], F32, **kw).ap()
    G.xmid_d = nc.dram_tensor("xmid_d", [S, D], F32, **kw).ap()
    G.km_d = nc.dram_tensor("km_d", [512, 32], F32, **kw).ap()

    with ExitStack() as es:
        P = Prog(nc, es)
        AR = Arena(nc, es, 45 * 1024)
        PB = []
        for i in range(8):
            t = es.enter_context(nc.psum_tensor('pb%d' % i, [128, 512], F32))
            PB.append(TT('pb%d' % i, t[:, :]))
        G.P, G.AR, G.PB, G.nc = P, AR, PB, nc
        final = []
        for l in range(n_layers):
            xin = G.x if l == 0 else G.xmid_d
            xin_name = 'x_in' if l == 0 else 'xmid'
            xout = G.xmid_d if l == 0 and n_layers > 1 else G.out
            xout_name = 'xmid' if l == 0 and n_layers > 1 else 'xout'
            if 'A' in stages:
                stage_A(G, l, xin, xin_name)
                P.barrier()
                AR.reset()
            if 'B' in stages:
                stage_B(G, l)
                P.barrier()
                AR.reset()
            if 'C' in stages:
                stage_C(G, l, xin, xin_name, xout, xout_name)
                P.barrier()
                AR.reset()
        final = [(id(P.dsem[j]), P.dcnt[j]) for j in range(len(P.dsem)) if P.dcnt[j] > 0]
        P.finish(final)
    return nc


def stage_A(G, l, xin, xin_name):
    P, AR, PB = G.P, G.AR, G.PB
    w_in = AR.alloc('w_in', [8, 5120], BF16)
    xs = AR.alloc('xs', [4, 1024], F32)
    xT = AR.alloc('xT', [8, 512], BF16)
    ident = AR.alloc('ident', [128], F32)
    ones = AR.alloc('ones', [128], BF16)
    bda = AR.alloc('bda', [4, 128], BF16)
    bdx = AR.alloc('bdx', [4, 128], BF16)
    pp = AR.alloc('pp', [NPP], F32)
    gaing = AR.alloc('gaing', [512], F32)
    cst = AR.alloc('cst', [8], F32)
    nsp8 = AR.alloc('nsp8', [4], F32)
    tmp4 = AR.alloc('tmp4', [4], F32)
    raw = AR.alloc('raw', [4, 516], F32)
    praw = AR.alloc('praw', [4, 516], F32)
    hcar = AR.alloc('hcar', [4], F32)
    kms = AR.alloc('kms', [4, 32], F32)
    xl2 = [AR.alloc('xl%d' % i, [512], F32) for i in range(2)]
    xlb2 = [AR.alloc('xlb%d' % i, [512], BF16) for i in range(2)]
    r_t = AR.alloc('r_t', [512], F32)
    i_t = AR.alloc('i_t', [512], F32)
    a_t = AR.alloc('a_t', [512], F32)
    q_t = AR.alloc('q_t', [512], F32)
    u_t = AR.alloc('u_t', [512], F32)
    yb = AR.alloc('yb', [4, 512], F32)
    sg = AR.alloc('sg', [4, 512], F32)
    scx = i_t
    sqb = AR.alloc('sqb', [2, 512], BF16)
    rt = AR.alloc('rt', [512], F32)
    rinv = AR.alloc('rinv', [512], F32)
    qo = AR.alloc('qo', [2, 512], BF16)
    ko = AR.alloc('ko', [2, 512], BF16)
    vo = AR.alloc('vo', [2, 512], BF16)
    go = AR.alloc('go', [2, 512], F32)
    zo = AR.alloc('zo', [2, 512], BF16)

    P.dma('sp', ident[:, :], dram(G.ident, 'c_ident'))
    P.dma('sp', pp[:, :], dram(G.pp[l], 'c_pp'))
    P.dma('sp', gaing[:, :], dram(bass.AP(G.bcp.tensor, l * (512 + 4 * D), [[0, 128], [1, 512]]), 'c_bcp'))
    stg = [yb.at(i)[:, i, :] for i in range(4)] + [sg.at(i)[:, i, :] for i in range(4)]
    lc = LoadCast(P, stg)
    for c in range(4):
        lc.go(bda[:, c, :], dram(G.bd_a[l, c], 'c_bda'), 128)
        lc.go(bdx[:, c, :], dram(G.bd_x[l, c], 'c_bdx'), 128)
    P.memset('pool', ones[:, :], 1.0)
    P.memset('pool', cst[:, 0:1], RMS_EPS)
    P.memset('pool', cst[:, 1:2], 1.0)
    P.memset('pool', raw[:, :, :], 0.0)
    P.memset('pool', praw[:, :, :], 0.0)
    P.memset('pool', hcar[:, :], 0.0)
    P.memset('pool', kms[:, :, :], 0.0)
    P.act(tmp4[:, :], pp[:, 28:32], AF.Exp, scale=-1.0)
    P.act(tmp4[:, :], tmp4[:, :], AF.Ln, bias=cst[:, 1:2])
    P.ts('dve', nsp8[:, :], tmp4[:, :], -8.0, None, ALU.mult)
    w_in_l = G.w_in[l].rearrange('(kc p) c -> p kc c', p=128)
    for s_ in (4, 0, 1, 2, 3, 5, 8, 7, 6, 9):
        for kc in range(8):
            lc.go(w_in.at(s_)[:, kc, s_ * 512:(s_ + 1) * 512],
                  dram(w_in_l[:, kc, s_ * 512:(s_ + 1) * 512], 'c_w_in'), 512)

    x_t = xin.rearrange('(t j p) d -> t p j d', j=4, p=128)

    def load_x(t):
        for j in range(4):
            P.dma('sp', xs.at(j)[:, j, :], dram(x_t[t][:, j, :], xin_name, t))

    bank_rr = [0]

    def nbank():
        b = PB[2 + bank_rr[0] % 6]
        bank_rr[0] += 1
        return b

    def fm(col0):
        b = nbank()
        s_ = col0 // 512
        for kc in range(8):
            P.mm(b[:, :], w_in.at(s_)[:, kc, col0:col0 + 128], xT[:, kc, :],
                 start=(kc == 0), stop=(kc == 7), inc=(kc == 7))
        return b

    def tm(col0, j):
        b = nbank()
        s_ = col0 // 512
        for kc in range(8):
            P.mm(b[:, :], xT[:, kc, j * 128:(j + 1) * 128], w_in.at(s_)[:, kc, col0:col0 + 512],
                 start=(kc == 0), stop=(kc == 7), inc=(kc == 7))
        return b

    def do_transposes(t):
        for kc in range(8):
            b = PB[kc % 2]
            for j in range(4):
                P.tr(b[:, j * 128:(j + 1) * 128], xs.at(j)[:, j, kc * 128:(kc + 1) * 128], ident[:, :], inc=(j == 3))
            if kc % 2 == 0:
                P.copy('act', xT[:, kc, :], b[:, :])
            else:
                P.copy('dve', xT[:, kc, :], b[:, :])
        if t + 1 < NT:
            load_x(t + 1)

    load_x(0)
    do_transposes(0)
    for t in range(NT):
        tok0 = t * 512
        def do_q(c):
            b = fm(0 + c * 128)
            P.ts('dve', qo.at(c % 2)[:, c % 2, :], b[:, :], 0.125, None, ALU.mult)
            P.dma('sp', dram(G.qT_d[c * 128:(c + 1) * 128, tok0:tok0 + 512], 'qT_d', t), qo.at(c % 2)[:, c % 2, :])

        def do_k(c):
            b = fm(512 + c * 128)
            for hb in range(2):
                P.act(ko.at(c % 2)[:, c % 2, hb * 256:(hb + 1) * 256], b[:, hb * 256:(hb + 1) * 256], AF.Copy,
                      accum=kms[:, c, 2 * t + hb:2 * t + hb + 1])
            P.dma('sp', dram(G.kT_d[c * 128:(c + 1) * 128, tok0:tok0 + 512], 'kT_d', t), ko.at(c % 2)[:, c % 2, :])

        def do_v(j):
            b = tm(1024, j)
            P.copy('dve', vo.at(j % 2)[:, j % 2, :], b[:, :])
            P.dma('sp', dram(G.V_d[tok0 + j * 128:tok0 + (j + 1) * 128, :], 'V_d', t), vo.at(j % 2)[:, j % 2, :])

        def do_g(j):
            b = tm(1536, j)
            P.act(go.at(j % 2)[:, j % 2, :], b[:, :], AF.Silu)
            P.tt('pool', go.at(j % 2)[:, j % 2, :], go.at(j % 2)[:, j % 2, :], gaing[:, :], ALU.mult)
            P.dma('sp', dram(G.gatt_d[tok0 + j * 128:tok0 + (j + 1) * 128, :], 'gatt_d', t), go.at(j % 2)[:, j % 2, :])

        def lru_front(c):
            xl_c = xl2[c % 2]
            b = fm(2048 + c * 128)
            P.copy('dve', raw.at(c)[:, c, 3:515], b[:, :])
            P.ts('dve', xl_c[:, :], raw.at(c)[:, c, 3:515], pp[:, c * 4 + 3:c * 4 + 4], pp[:, 16 + c:17 + c], ALU.mult, ALU.add)
            for k in range(3):
                P.stt(xl_c[:, :], raw.at(c)[:, c, k:k + 512], pp[:, c * 4 + k:c * 4 + k + 1], xl_c[:, :], ALU.mult, ALU.add)
            P.copy('pool', raw.at(c)[:, c, 0:3], raw.at(c)[:, c, 512:515])
            P.copy('pool', xlb2[c % 2][:, :], xl_c[:, :])

        def lru_chain(c):
            xl_c = xl2[c % 2]
            xlb_c = xlb2[c % 2]
            bg = nbank()
            P.mm(bg[:, :], bda[:, c, :], xlb_c[:, :])
            P.act(r_t[:, :], bg[:, :], AF.Sigmoid, bias=pp[:, 20 + c:21 + c])
            bg2 = nbank()
            P.mm(bg2[:, :], bdx[:, c, :], xlb_c[:, :])
            P.act(i_t[:, :], bg2[:, :], AF.Sigmoid, bias=pp[:, 24 + c:25 + c])
            P.act(a_t[:, :], r_t[:, :], AF.Exp, scale=nsp8[:, c:c + 1])
            P.act(q_t[:, :], a_t[:, :], AF.Square)
            P.act(q_t[:, :], q_t[:, :], AF.Sqrt, bias=cst[:, 1:2], scale=-1.0)
            P.tt('pool', u_t[:, :], i_t[:, :], xl_c[:, :], ALU.mult)
            P.tt('dve', u_t[:, :], u_t[:, :], q_t[:, :], ALU.mult)
            P.scan(yb.at(c)[:, c, :], a_t[:, :], u_t[:, :], hcar.at(c)[:, c:c + 1])
            P.copy('pool', hcar.at(c)[:, c:c + 1], yb.at(c)[:, c, 511:512])

        for c in range(4):
            lru_front(c)
            do_q(c)
            do_k(c)
            do_v(c)
            if c >= 1:
                lru_chain(c - 1)
            do_g(c)
        lru_chain(3)

        def norm1a(gate_col0):
            for c in range(4):
                b = fm(gate_col0 + c * 128)
                P.act(sg.at(c)[:, c, :], b[:, :], AF.Silu)

        def norm1(gate_col0):
            norm1a(gate_col0)
            norm1b()

        def norm1b():
            bs = nbank()
            for c in range(4):
                P.tt('pool', sqb.at(c % 2)[:, c % 2, :], yb.at(c)[:, c, :], yb.at(c)[:, c, :], ALU.mult)
                P.mm(bs[:, :], ones[:, :], sqb.at(c % 2)[:, c % 2, :], start=(c == 0), stop=(c == 3), inc=True)
            P.act(rt[:, :], bs[:, :], AF.Sqrt, bias=cst[:, 0:1], scale=1.0 / 512.0)
            P.recip(rinv[:, :], rt[:, :])

        def norm2(grp, gain_col):
            for c in range(4):
                P.stt(yb.at(c)[:, c, :], yb.at(c)[:, c, :], pp[:, gain_col + c:gain_col + c + 1], rinv[:, :], ALU.mult, ALU.mult)
                P.tt('pool', zo.at(c % 2)[:, c % 2, :], yb.at(c)[:, c, :], sg.at(c)[:, c, :], ALU.mult)
                r0 = (grp - 1) * 512 + c * 128
                P.dma('sp', dram(G.zT_d[r0:r0 + 128, tok0:tok0 + 512], 'zT_d', (grp, t)), zo.at(c % 2)[:, c % 2, :])

        def sc_front(c):
            bx = fm(4096 + c * 128)
            P.copy('dve', scx[:, :], bx[:, :])
            bc_ = fm(3584 + c * 128)
            P.tt('dve', praw.at(c)[:, c, 2:514], bc_[:, :], scx[:, :], ALU.mult)
            xc = xl2[c % 2]
            P.ts('dve', xc[:, :], praw.at(c)[:, c, 2:514], pp[:, 32 + c * 3 + 2:32 + c * 3 + 3], None, ALU.mult)
            for k in range(2):
                P.stt(xc[:, :], praw.at(c)[:, c, k:k + 512], pp[:, 32 + c * 3 + k:32 + c * 3 + k + 1], xc[:, :], ALU.mult, ALU.add)
            P.copy('pool', praw.at(c)[:, c, 0:2], praw.at(c)[:, c, 512:514])

        def sc_back(c):
            bb = fm(3072 + c * 128)
            P.tt('dve', yb.at(c)[:, c, :], bb[:, :], xl2[c % 2][:, :], ALU.mult)

        norm1(2560)
        sc_front(0)
        norm2(1, 44)
        sc_back(0)
        for c in range(1, 4):
            sc_front(c)
            sc_back(c)
        norm1a(4608)
        if t + 1 < NT:
            do_transposes(t + 1)
        norm1b()
        norm2(2, 48)
    for c in range(4):
        P.dma('sp', dram(G.km_d[c * 128:(c + 1) * 128, :], 'km_d'), kms[:, c, :])


def group_norm_out(G, l, t, grp, fm, gate_col0, yb, sg, sqb, rt, rinv, zo, cst, ones, pp, gain_col, nbank):
    P = G.P
    tok0 = t * 512
    for c in range(4):
        b = fm(gate_col0 + c * 128)
        P.act(sg.at(c)[:, c, :], b[:, :], AF.Silu)
    bs = nbank()
    for c in range(4):
        P.act(sqb.at(c % 2)[:, c % 2, :], yb.at(c)[:, c, :], AF.Square)
        P.mm(bs[:, :], ones[:, :], sqb.at(c % 2)[:, c % 2, :], start=(c == 0), stop=(c == 3), inc=True)
    P.act(rt[:, :], bs[:, :], AF.Sqrt, bias=cst[:, 0:1], scale=1.0 / 512.0)
    P.recip(rinv[:, :], rt[:, :])
    for c in range(4):
        P.stt(yb.at(c)[:, c, :], yb.at(c)[:, c, :], pp[:, gain_col + c:gain_col + c + 1], rinv[:, :], ALU.mult, ALU.mult)
        P.tt('pool', zo.at(c % 2)[:, c % 2, :], yb.at(c)[:, c, :], sg.at(c)[:, c, :], ALU.mult)
        r0 = (grp - 1) * 512 + c * 128
        P.dma('sp', dram(G.zT_d[r0:r0 + 128, tok0:tok0 + 512], 'zT_d', (grp, t)), zo.at(c % 2)[:, c % 2, :])


def stage_B(G, l):
    P, AR, PB = G.P, G.AR, G.PB
    ident = AR.alloc('identB', [128], F32)
    identb = AR.alloc('identb', [128], BF16)
    trib = AR.alloc('trib', [2, 256], BF16)
    vb1 = AR.alloc('vb1', [2048], F32)
    vb2 = AR.alloc('vb2', [2048], F32)
    kmf = AR.alloc('kmf', [4, 32], F32)
    kmb = AR.alloc('kmb', [8, 32], BF16)
    stg = AR.alloc('stgB', [2048], F32)
    qa = [AR.alloc('qa%d' % i, [S], BF16) for i in range(2)]
    ka = [AR.alloc('ka%d' % i, [S], BF16) for i in range(2)]
    va = [AR.alloc('va%d' % i, [64, 65], BF16) for i in range(2)]
    gm = AR.alloc('gm', [16, 32], F32)
    gm2 = AR.alloc('gm2', [16, 32], F32)
    top8 = AR.alloc('top8', [16, 8], F32)
    mbp = AR.alloc('mbp', [16, 96], F32)
    pT = [AR.alloc('pT%d' % i, [512], BF16) for i in range(4)]
    yo = AR.alloc('yo', [2, 4, 64], F32)
    rec = AR.alloc('rec', [2, 4], F32)

    P.dma('sp', ident[:, :], dram(G.ident, 'c_ident'))
    P.copy('dve', identb[:, :], ident[:, :])
    for r in range(2):
        P.dma('sp', stg[:, 0:256], dram(G.trib[r], 'c_trib'))
        P.copy('dve', trib[:, r, :], stg[:, 0:256])
    P.dma('sp', vb1[:, :], dram(bass.AP(G.vb.tensor, 0, [[0, 128], [1, 2048]]), 'c_vb'))
    P.dma('sp', vb2[:, :], dram(bass.AP(G.vb.tensor, 2048, [[0, 128], [1, 2048]]), 'c_vb'))
    for c in range(4):
        P.dma('sp', kmf[:, c, :], dram(G.km_d[c * 128:(c + 1) * 128, :], 'km_d'))
    P.memset('pool', mbp[:, :, :], 0.0)
    for i in range(2):
        P.memset('pool', va[i].at('one')[:, :, 64:65], 1.0)
        for c4 in range(4):
            sv = V(stg.ap[64:96, :], stg[:, :].key)
            P.dma('sp', sv, dram(G.E[:, c4 * 2048:(c4 + 1) * 2048], 'c_E'))
            P.copy('dve', V(ka[i].ap[64:96, c4 * 2048:(c4 + 1) * 2048], (ka[i].name, 'E')), sv)
    for h in range(8):
        par = h % 2
        P.copy('dve', V(kmb.ap[0:64, h, :], (kmb.name, h)), V(kmf.ap[64 * par:64 * par + 64, h // 2, :], (kmf.name, None)))

    v_r = G.V_d.rearrange('(t p) c -> p t c', p=128)
    y_r = G.yatt_d.rearrange('(t j p) c -> t p j c', j=4, p=128)

    def load_head(h):
        i = h % 2
        P.dma('sp', V(qa[i].ap[0:64, :], (qa[i].name, 'q')), dram(G.qT_d[64 * h:64 * h + 64, :], 'qT_d'))
        P.dma('sp', V(ka[i].ap[0:64, :], (ka[i].name, 'k')), dram(G.kT_d[64 * h:64 * h + 64, :], 'kT_d'))
        for g in range(4):
            P.dma('sp', V(va[i].ap[:, g * 16:(g + 1) * 16, 0:64], (va[i].name, 'v')),
                  dram(v_r[:, g * 16:(g + 1) * 16, 64 * h:64 * h + 64], 'V_d'))

    def gate_A(h, g16):
        i = h % 2
        if True:
            for s16 in range(16):
                sub = g16 * 16 + s16
                P.mm(PB[6][:, s16 * 32:(s16 + 1) * 32],
                     V(qa[i].ap[0:64, sub * 128:(sub + 1) * 128], (qa[i].name, 'q')),
                     V(kmb.ap[0:64, h, :], (kmb.name, h)), start=True, stop=True, inc=(s16 == 15))
            gmf = V(gm.ap[:, :, :].rearrange('p a b -> p (a b)'), gm[:, :, :].key)
            gm2f = V(gm2.ap[:, :, :].rearrange('p a b -> p (a b)'), gm2[:, :, :].key)
            P.tt('dve', gmf, PB[6][:, :], vb1[:, g16 * 512:(g16 + 1) * 512], ALU.add)
            P.tt('dve', gm2f, PB[6][:, :], vb2[:, g16 * 512:(g16 + 1) * 512], ALU.add)
            for s16 in range(16):
                P.max8(top8.at(s16)[:, s16, :], gm[:, s16, :])
            for s16 in range(16):
                P.ts('dve', mbp.at(s16)[:, s16, 64:96], gm2[:, s16, :], top8.at(s16)[:, s16, 2:3], -BIG, ALU.is_lt, ALU.mult)
    def gate_B(h, g16, dve_only=False):
        i = h % 2
        if True:
            for g4 in range(4):
                for s4 in range(4):
                    s16 = g4 * 4 + s4
                    P.tr(V(PB[7].ap[0:96, s4 * 128:(s4 + 1) * 128], PB[7][:, :].key), mbp.at(s16)[:, s16, 0:96], ident[:, :], inc=(s4 == 3))
                sub0 = g16 * 16 + g4 * 4
                eng = 'act' if (g4 % 2 == 0 and not dve_only) else 'dve'
                P.copy(eng, V(qa[i].ap[64:96, sub0 * 128:sub0 * 128 + 512], (qa[i].name, 'm')),
                       V(PB[7].ap[64:96, :], PB[7][:, :].key))

    def gate_phase(h):
        for g16 in range(4):
            gate_A(h, g16)
            gate_B(h, g16)

    def main_phase(h, hooks=None):
        i = h % 2
        qkeys = [(qa[i].name, 'q'), (qa[i].name, 'm')]
        kkeys = [(ka[i].name, 'k'), (ka[i].name, 'E')]
        vkeys = [(va[i].name, 'v'), (va[i].name, 'one')]
        units = []
        for qt in range(NT):
            for kt in range(4 * (qt + 1)):
                units.append((qt, kt))
        N = len(units)
        DEPTH = 2

        def emit_S(n):
            qt, kt = units[n]
            i_d = kt - 4 * qt
            c0 = 128 * i_d if i_d > 0 else 0
            sb = PB[2 + n % 4]
            P.mm(sb[:, c0:512], V(ka[i].ap[0:96, kt * 128:(kt + 1) * 128], kkeys),
                 V(qa[i].ap[0:96, qt * 512 + c0:(qt + 1) * 512], qkeys), start=True, stop=(i_d < 0), inc=(i_d < 0))
            if i_d >= 0:
                m, r = i_d // 2, i_d % 2
                lo, hi = max(c0, 256 * m), 256 * m + 256
                P.mm(sb[:, lo:hi], identb[:, :], trib[:, r, lo - 256 * m:256], start=False, stop=True, inc=True)
            P.act(pT[n % 4][:, c0:512], sb[:, c0:512], AF.Exp)

        def emit_PV(n):
            qt, kt = units[n]
            i_d = kt - 4 * qt
            c0 = 128 * i_d if i_d > 0 else 0
            acc = PB[qt % 2]
            last_kt = (kt == 4 * (qt + 1) - 1)
            for j in range(4):
                if 128 * j < c0:
                    continue
                P.mm(acc[:, j * 128:j * 128 + 65], pT[n % 4][:, j * 128:(j + 1) * 128],
                     V(va[i].ap[:, kt, 0:65], vkeys), start=(kt == 0 and j == 0), stop=(last_kt and j == 3),
                     inc=(j == 3))
            if last_kt:
                sl = qt % 2
                accv = acc.ap[:, :].rearrange('p (j c) -> p j c', j=4)
                P.recip(V(rec.ap[:, sl, :].unsqueeze(2), (rec.name, sl)), V(accv[:, :, 64:65], acc[:, :].key))
                P.tt('dve', yo.at(sl)[:, sl, :, :], V(accv[:, :, 0:64], acc[:, :].key),
                     V(rec.ap[:, sl, :].unsqueeze(2).broadcast_to([128, 4, 64]), (rec.name, sl)), ALU.mult)
                P.dma('sp', dram(y_r[qt][:, :, 64 * h:64 * h + 64], 'yatt_d', (h, qt)), yo.at(sl)[:, sl, :, :])

        for n in range(N + DEPTH):
            if hooks and n in hooks:
                hooks[n]()
            if n < N:
                emit_S(n)
            if n - DEPTH >= 0:
                emit_PV(n - DEPTH)

    load_head(0)
    gate_phase(0)
    for h in range(NHEAD_RUN):
        hooks = None
        if h + 1 < NHEAD_RUN:
            load_head(h + 1)
            hooks = {}
            for g16 in range(4):
                hooks[30 + 120 * g16] = (lambda hh=h + 1, g=g16: gate_A(hh, g))
                hooks[95 + 120 * g16] = (lambda hh=h + 1, g=g16: gate_B(hh, g, dve_only=True))
        main_phase(h, hooks)


def alias(tt, shape, dt):
    ap = tt.ap
    nd = len(ap.shape)
    if nd == 3:
        ap = ap.rearrange('p a b -> p (a b)')
    elif nd == 4:
        ap = ap.rearrange('p a b c -> p (a b c)')
    if ap.dtype != dt:
        ap = ap.bitcast(dt)
    n = 1
    for s_ in shape:
        n *= s_
    ap = ap[:, 0:n]
    if len(shape) == 2:
        ap = ap.rearrange('p (a b) -> p a b', a=shape[0])
    elif len(shape) == 3:
        ap = ap.rearrange('p (a b c) -> p a b c', a=shape[0], b=shape[1])
    return TT(tt.name, ap)


def stage_C(G, l, xin, xin_name, xout, xout_name):
    P, AR, PB = G.P, G.AR, G.PB
    w_out = AR.alloc('w_out', [12, 1024], BF16)
    wq = AR.alloc('wq', [8, 1024], BF16)
    wo = AR.alloc('wo', [8, 1024], BF16)
    KmT = AR.alloc('KmT', [8, 256], BF16)
    Vm = AR.alloc('Vm', [2, 4, 258], BF16)
    ident = AR.alloc('identC', [128], F32)
    gbc = AR.alloc('gbc', [4, 1024], F32)
    cst = AR.alloc('cstC', [8], F32)
    xres = [AR.alloc('xres%d' % i, [4, 1024], F32) for i in range(2)]
    bufAB = AR.alloc('bufAB', [4, 1024], F32)
    zT = AR.alloc('zT', [12, 512], BF16)
    x1T = AR.alloc('x1T', [8, 512], BF16)
    qT = AR.alloc('qTc', [8, 512], BF16)
    pT = [[AR.alloc('pTc%d_%d' % (i, m), [512], BF16) for m in range(2)] for i in range(2)]
    ss = AR.alloc('ss', [4], F32)
    rt4 = AR.alloc('rt4', [4], F32)
    r4 = AR.alloc('r4', [4], F32)
    st = AR.alloc('st', [4, 2, 6], F32)
    mv = AR.alloc('mv', [4, 2], F32)
    sd = AR.alloc('sd', [4], F32)
    rstd = AR.alloc('rstd', [4], F32)
    nb_ = AR.alloc('nb', [4], F32)
    rc = AR.alloc('rc', [8], F32)
    oT = alias(x1T, [8, 512], BF16)
    junk = alias(qT, [512], F32)
    obuf = AR.alloc('obuf', [4, 1024], F32)
    wk_v = alias(bufAB, [8, 1024], BF16)
    wv_v = alias(xres[0], [8, 1024], BF16)
    memT = alias(x1T, [8, 256], BF16)

    P.dma('sp', ident[:, :], dram(G.ident, 'c_ident'))
    for i in range(4):
        P.dma('sp', gbc[:, i, :], dram(bass.AP(G.bcp.tensor, l * (512 + 4 * D) + 512 + i * D, [[0, 128], [1, D]]), 'c_bcp'))
    P.memset('pool', cst[:, 0:1], RMS_EPS)
    P.memset('pool', cst[:, 1:2], LN_EPS)
    P.memset('pool', Vm[:, :, :, 256:257], 1.0)
    stg = [V(xres[1].ap[:, j, h2 * 512:(h2 + 1) * 512], (xres[1].name, (j, h2))) for j in range(4) for h2 in range(2)]
    lc = LoadCast(P, stg)

    def load_w(dst, src2d, nk):
        src = src2d.rearrange('(kc p) c -> p kc c', p=128)
        for kc in range(nk):
            for h2 in range(2):
                lc.go(dst[:, kc, h2 * 512:(h2 + 1) * 512], dram(src[:, kc, h2 * 512:(h2 + 1) * 512], 'c_w'), 512)

    mt = G.memT.rearrange('(kc p) m -> p kc m', p=128)
    for kc in range(8):
        lc.go(memT[:, kc, :], dram(mt[:, kc, :], 'c_mem'), 256)
    load_w(wk_v, G.wk[l], 8)
    load_w(wv_v, G.wv[l], 8)
    rr = [0]

    def nbank():
        b = PB[2 + rr[0] % 6]
        rr[0] += 1
        return b

    ev = [0]

    def evac(out, in_, scale=None):
        ev[0] += 1
        if scale is not None:
            P.act(out, in_, AF.Copy, scale=scale)
        elif ev[0] % 2 == 0:
            P.copy('act', out, in_)
        else:
            P.copy('dve', out, in_)

    for oc in range(8):
        b = nbank()
        for kc in range(8):
            P.mm(b[:, 0:256], wk_v[:, kc, oc * 128:(oc + 1) * 128], memT[:, kc, :], start=(kc == 0), stop=(kc == 7), inc=(kc == 7))
        evac(KmT[:, oc, :], b[:, 0:256])
    for mh in range(2):
        for half in range(2):
            b = nbank()
            for kc in range(8):
                P.mm(b[:, :], memT[:, kc, mh * 128:(mh + 1) * 128], wv_v[:, kc, half * 512:(half + 1) * 512],
                     start=(kc == 0), stop=(kc == 7), inc=(kc == 7))
            evac(Vm[:, mh, 2 * half:2 * half + 2, 0:256], V(b.ap[:, :].rearrange('p (a b) -> p a b', a=2), b[:, :].key))
    load_w(w_out, G.w_out[l], 12)
    load_w(wq, G.wq[l], 8)
    load_w(wo, G.wo[l], 8)

    x_t = xin.rearrange('(t j p) d -> t p j d', j=4, p=128)
    o_t = xout.rearrange('(t j p) d -> t p j d', j=4, p=128)
    ya_t = G.yatt_d.rearrange('(t j p) c -> t p j c', j=4, p=128)
    ga_t = G.gatt_d.rearrange('(t j p) c -> t p j c', j=4, p=128)
    zT_r = G.zT_d.rearrange('(kc p) t -> p kc t', p=128)

    def ln_p1(xr, j):
        for h2 in range(2):
            P.bn_stats(st.at(j)[:, j, h2, :], xr.at(j)[:, j, h2 * 512:(h2 + 1) * 512])
        P.bn_aggr(mv.at(j)[:, j, :], V(st.ap[:, j, :, :].rearrange('p a b -> p (a b)'), (st.name, j)))
        P.act(sd.at(j)[:, j:j + 1], mv.at(j)[:, j, 1:2], AF.Sqrt, bias=cst[:, 1:2])

    def ln_p2(xr, j):
        P.recip(rstd.at(j)[:, j:j + 1], sd.at(j)[:, j:j + 1])
        P.ts('dve', nb_.at(j)[:, j:j + 1], mv.at(j)[:, j, 0:1], -1.0, rstd.at(j)[:, j:j + 1], ALU.mult, ALU.mult)
        P.act(xr.at(j)[:, j, :], xr.at(j)[:, j, :], AF.Identity, bias=nb_.at(j)[:, j:j + 1], scale=rstd.at(j)[:, j:j + 1])

    def ln_p3(xr, j, gi):
        P.tt('dve', xr.at(j)[:, j, :], xr.at(j)[:, j, :], gbc[:, gi, :], ALU.mult)
        P.tt('pool', xr.at(j)[:, j, 0:512], xr.at(j)[:, j, 0:512], gbc[:, gi + 1, 0:512], ALU.add)
        P.tt('dve', xr.at(j)[:, j, 512:1024], xr.at(j)[:, j, 512:1024], gbc[:, gi + 1, 512:1024], ALU.add)

    def ln_pipeline(xr, gi, mm_fn, after_fn, mid_fn=None, mid_early=False):
        if mid_early:
            sched = [('1', 0), ('1', 1), ('M', 0), ('2', 0), ('1', 2), ('2', 1), ('3', 0), ('1', 3), ('A', 0), ('2', 2),
                     ('3', 1), ('A', 1), ('2', 3), ('3', 2), ('A', 2), ('3', 3), ('A', 3)]
        else:
            sched = [('1', 0), ('1', 1), ('2', 0), ('1', 2), ('2', 1), ('3', 0), ('1', 3), ('M', 0), ('A', 0), ('2', 2),
                     ('3', 1), ('A', 1), ('2', 3), ('3', 2), ('A', 2), ('3', 3), ('A', 3)]
        for ph, j in sched:
            if ph == '1':
                mm_fn(j)
                ln_p1(xr, j)
            elif ph == '2':
                ln_p2(xr, j)
            elif ph == '3':
                ln_p3(xr, j, gi)
            elif ph == 'M':
                if mid_fn is not None:
                    mid_fn()
            else:
                after_fn(j)

    tr_rr = [0]

    def transposes_j(src_fn, dst, j, nkc):
        for g in range(nkc // 4):
            b = PB[tr_rr[0] % 2]
            tr_rr[0] += 1
            for k4 in range(4):
                P.tr(b[:, k4 * 128:(k4 + 1) * 128], src_fn(j, g * 4 + k4), ident[:, :], inc=(k4 == 3))
            evac(V(dst.ap[:, g * 4:(g + 1) * 4, j * 128:(j + 1) * 128], dst[:, :, :].key),
                 V(b.ap[:, :].rearrange('p (a b) -> p a b', a=4), b[:, :].key))

    def transposes(src_fn, dst, nkc):
        for kc in range(nkc):
            b = PB[kc % 2]
            for j in range(4):
                P.tr(b[:, j * 128:(j + 1) * 128], src_fn(j, kc), ident[:, :], inc=(j == 3))
            evac(dst[:, kc, :], b[:, :])

    def load_tile(T):
        xr = xres[T % 2]
        for j in range(4):
            P.dma('sp', xr.at(j)[:, j, :], dram(x_t[T][:, j, :], xin_name, T))
        P.dma('sp', bufAB[:, :, 0:512], dram(ya_t[T], 'yatt_d'))
        P.dma('sp', bufAB[:, :, 512:1024], dram(ga_t[T], 'gatt_d'))

    def load_tile_b(T):
        P.dma('sp', zT[:, 4:12, :], dram(zT_r[:, :, T * 512:(T + 1) * 512], 'zT_d'))

    P.memset('pool', xres[1][:, 0, 0:1], 0.0)
    def att_norm():
        for j in range(4):
            P.act(junk[:, :], bufAB[:, j, 0:512], AF.Square, accum=ss.at(j)[:, j:j + 1])
        P.act(rt4[:, :], ss[:, :], AF.Sqrt, bias=cst[:, 0:1], scale=1.0 / 512.0)
        P.recip(r4[:, :], rt4[:, :])
        for j in range(4):
            P.stt(bufAB[:, j, 0:512], bufAB[:, j, 0:512], r4[:, j:j + 1], bufAB[:, j, 512:1024], ALU.mult, ALU.mult)

    def z_att_tr():
        transposes(lambda j, kc: bufAB[:, j, kc * 128:(kc + 1) * 128], zT, 4)

    load_tile(0)
    load_tile_b(0)
    att_norm()
    z_att_tr()
    for T in range(NT):
        xr = xres[T % 2]
        if T + 1 < NT:
            load_tile(T + 1)
        def mm_wout(j, xr=xr):
            for h2 in range(2):
                b = nbank()
                for kc in range(12):
                    P.mm(b[:, :], zT[:, kc, j * 128:(j + 1) * 128], w_out[:, kc, h2 * 512:(h2 + 1) * 512],
                         start=(kc == 0), stop=(kc == 11), inc=(kc == 11))
                P.stt(xr.at(j)[:, j, h2 * 512:(h2 + 1) * 512], xr.at(j)[:, j, h2 * 512:(h2 + 1) * 512], ALPHA, b[:, :], ALU.mult, ALU.add)

        ln_pipeline(xr, 0, mm_wout,
                    lambda j, xr=xr: transposes_j(lambda jj, kc: xr.at(jj)[:, jj, kc * 128:(kc + 1) * 128], x1T, j, 8),
                    mid_fn=(lambda T=T: load_tile_b(T + 1)) if T + 1 < NT else None)
        for oc in range(8):
            b = nbank()
            for kc in range(8):
                P.mm(b[:, :], wq[:, kc, oc * 128:(oc + 1) * 128], x1T[:, kc, :], start=(kc == 0), stop=(kc == 7), inc=(kc == 7))
            evac(qT[:, oc, :], b[:, :], scale=1.0 / 16.0)

        def emit_S(hd):
            pb_ = pT[hd % 2]
            for mh in range(2):
                b = nbank()
                for cc in range(2):
                    P.mm(b[:, :], KmT[:, 2 * hd + cc, mh * 128:(mh + 1) * 128], qT[:, 2 * hd + cc, :],
                         start=(cc == 0), stop=(cc == 1), inc=(cc == 1))
                P.act(pb_[mh][:, :], b[:, :], AF.Exp)

        def emit_PV(hd):
            pb_ = pT[hd % 2]
            for j in range(4):
                b = nbank()
                for mh in range(2):
                    P.mm(b[:, 0:257], pb_[mh][:, j * 128:(j + 1) * 128], Vm[:, mh, hd, 0:257],
                         start=(mh == 0), stop=(mh == 1), inc=(mh == 1))
                k8 = (hd * 4 + j) % 8
                P.recip(rc.at(k8)[:, k8:k8 + 1], b[:, 256:257])
                P.ts('dve', obuf[:, j, hd * 256:(hd + 1) * 256], b[:, 0:256], rc.at(k8)[:, k8:k8 + 1], None, ALU.mult)

        emit_S(0)
        for hd in range(4):
            if hd + 1 < 4:
                emit_S(hd + 1)
            emit_PV(hd)
        transposes(lambda j, kc: obuf[:, j, kc * 128:(kc + 1) * 128], oT, 8)
        if T + 1 < NT:
            att_norm()
        def mm_wo(j, xr=xr):
            for h2 in range(2):
                b = nbank()
                for kc in range(8):
                    P.mm(b[:, :], oT[:, kc, j * 128:(j + 1) * 128], wo[:, kc, h2 * 512:(h2 + 1) * 512],
                         start=(kc == 0), stop=(kc == 7), inc=(kc == 7))
                P.stt(xr.at(j)[:, j, h2 * 512:(h2 + 1) * 512], xr.at(j)[:, j, h2 * 512:(h2 + 1) * 512], ALPHA, b[:, :], ALU.mult, ALU.add)

        ln_pipeline(xr, 2, mm_wo,
                    lambda j, xr=xr, T=T: P.dma('sp', dram(o_t[T][:, j, :], xout_name, T), xr.at(j)[:, j, :]),
                    mid_fn=z_att_tr if T + 1 < NT else None, mid_early=True)


def host_constants():
    ident = np.eye(128, dtype=np.float32)
    E = np.zeros((32, S), np.float32)
    for j in range(32):
        E[j, j * 256:(j + 1) * 256] = 1.0
    trib = np.zeros((2, 128, 256), np.float32)
    kk = np.arange(128)[:, None]
    qq = np.arange(256)[None, :]
    trib[0] = np.where(qq >= kk, 0.0, -BIG)
    trib[1] = np.where(qq >= kk + 128, 0.0, -BIG)
    vb = np.zeros((2, 1, 64, 32), np.float32)
    for sub in range(64):
        own = sub // 2
        for blk in range(32):
            if blk < own:
                vb[0, 0, sub, blk] = 0.0
                vb[1, 0, sub, blk] = 0.0
            elif blk == own:
                vb[0, 0, sub, blk] = -BIG
                vb[1, 0, sub, blk] = BIG
            else:
                vb[0, 0, sub, blk] = -BIG
                vb[1, 0, sub, blk] = -2 * BIG
    return ident, E, trib, vb.reshape(2, 1, 64 * 32)


def host_layout(inputs):
    f = lambda n: np.ascontiguousarray(np.asarray(inputs[n], dtype=np.float32))
    lw_a, lw_x = f('lru_w_a'), f('lru_w_x')
    bd_a = np.zeros((L, 4, 128, 128), np.float32)
    bd_x = np.zeros((L, 4, 128, 128), np.float32)
    for l in range(L):
        for c in range(4):
            for hh in range(2):
                bd_a[l, c, hh * 64:(hh + 1) * 64, hh * 64:(hh + 1) * 64] = lw_a[l, 2 * c + hh]
                bd_x[l, c, hh * 64:(hh + 1) * 64, hh * 64:(hh + 1) * 64] = lw_x[l, 2 * c + hh]
    pp = np.zeros((L, 128, NPP), np.float32)
    lcw, lcb, b_a, b_x, lam = f('lru_conv_w'), f('lru_conv_b'), f('lru_b_a'), f('lru_b_x'), f('lru_lambda')
    scw, gg = f('sc_conv_w'), f('group_gain')
    for l in range(L):
        for c in range(4):
            sl = slice(c * 128, (c + 1) * 128)
            for k in range(4):
                pp[l, :, c * 4 + k] = lcw[l, k, sl]
            pp[l, :, 16 + c] = lcb[l, sl]
            pp[l, :, 20 + c] = b_a[l, sl]
            pp[l, :, 24 + c] = b_x[l, sl]
            pp[l, :, 28 + c] = lam[l, sl]
            for k in range(3):
                pp[l, :, 32 + c * 3 + k] = scw[l, k, sl]
            pp[l, :, 44 + c] = gg[l, 1, sl]
            pp[l, :, 48 + c] = gg[l, 2, sl]
    bcp = np.concatenate([gg[:, 0, :], f('ln1_g'), f('ln1_b'), f('ln2_g'), f('ln2_b')], axis=1).reshape(L, 1, 512 + 4 * D)
    ident, E, trib, vb = host_constants()
    shared = dict(w_in=f('w_in'), w_out=f('w_out'), wq=f('xq_w'), wk=f('xk_w'), wv=f('xv_w'), wo=f('xo_w'),
                  bd_a=bd_a, bd_x=bd_x, pp=pp, bcp=np.ascontiguousarray(bcp), ident=ident, E=E, trib=trib, vb=vb)
    x, mem = f('x'), f('mem')
    maps = []
    for b in range(x.shape[0]):
        m = dict(shared)
        m['x'] = np.ascontiguousarray(x[b])
        m['memT'] = np.ascontiguousarray(mem[b].T)
        maps.append(m)
    return maps


_NC_CACHE = {}


def kernel(**inputs):
    maps = host_layout(inputs)
    if 'nc' not in _NC_CACHE:
        _NC_CACHE['nc'] = build_program()
    nc = _NC_CACHE['nc']
    B = len(maps)
    in_maps = [maps[i % B] for i in range(8)]
    res = run_bass_kernel_spmd(nc, in_maps, core_ids=list(range(8)))
    out = np.stack([np.asarray(res.results[b]['out'], dtype=np.float32) for b in range(B)], axis=0)
    return out
```

```python
from contextlib import ExitStack
import numpy as np
import concourse.bass as bass
import concourse.mybir as mybir
from concourse.bass_utils import run_bass_kernel_spmd

F32 = mybir.dt.float32
BF16 = mybir.dt.bfloat16
ALU = mybir.AluOpType
AF = mybir.ActivationFunctionType
AX = mybir.AxisListType

S = 8192
D = 1024
L = 2
NMEM = 256
NT = S // 512
ALPHA = float((2.0 * L) ** 0.25)
BIG = 1000.0
NPP = 52
STOP = None
NHEAD_RUN = 8
LN_EPS = 1e-5
RMS_EPS = 1e-6

ENGS = ['pe', 'act', 'dve', 'pool', 'sp']


class V:
    __slots__ = ('ap', 'key')

    def __init__(self, ap, key):
        self.ap = ap
        self.key = key


class _Slot:
    def __init__(self, t, slot):
        self.t = t
        self.slot = slot

    def __getitem__(self, idx):
        return V(self.t.ap[idx], (self.t.name, self.slot))


class TT:
    def __init__(self, name, ap):
        self.name = name
        self.ap = ap

    def __getitem__(self, idx):
        return V(self.ap[idx], (self.name, None))

    def at(self, slot):
        return _Slot(self, slot)


class Prog:
    def __init__(self, nc, es, n_dma_sems=32):
        self.nc = nc
        self.q = {e: [] for e in ENGS}
        self.sem = {e: es.enter_context(nc.semaphore('s_' + e)) for e in ENGS}
        self.cnt = {e: 0 for e in ENGS}
        self.dsem = [es.enter_context(nc.semaphore('d%d' % i)) for i in range(n_dma_sems)]
        self.dcnt = [0] * n_dma_sems
        self.drr = 0
        self.waited = {e: {} for e in ENGS}
        self.state = {}
        self.sems_by_id = {}
        self.n_wait = 0
        self.n_inst = 0
        for e in ENGS:
            self._semid(self.sem[e])
        for s in self.dsem:
            self._semid(s)

    def _semid(self, s):
        i = id(s)
        self.sems_by_id[i] = s
        return i

    def _slots(self, key):
        name, slot = key
        d = self.state.setdefault(name, {})
        if slot is None:
            if None not in d:
                d[None] = {'w': None, 'r': []}
            return list(d.values())
        if slot not in d:
            d[slot] = {'w': None, 'r': []}
        out = [d[slot]]
        if None in d:
            out.append(d[None])
        return out

    def _need(self, toks, reads, writes):
        for k in reads:
            for st in self._slots(k):
                if st['w'] is not None:
                    toks.append(st['w'])
        for k in writes:
            for st in self._slots(k):
                if st['w'] is not None:
                    toks.append(st['w'])
                toks.extend(st['r'])

    def _update(self, tok, reads, writes):
        for (name, slot) in reads:
            st = self.state[name][slot]
            st['r'] = [t for t in st['r'] if t[0] != tok[0]] + [tok]
        for (name, slot) in writes:
            d = self.state[name]
            if slot is None:
                for s in list(d.keys()):
                    d[s] = {'w': tok, 'r': []}
            else:
                d[slot] = {'w': tok, 'r': []}

    def _emit_waits(self, eng, toks):
        best = {}
        for (sid, v) in toks:
            if v > best.get(sid, 0):
                best[sid] = v
        w = self.waited[eng]
        for sid, v in best.items():
            if w.get(sid, 0) >= v:
                continue
            w[sid] = v
            s = self.sems_by_id[sid]
            self.q[eng].append(lambda e, s=s, v=v: e.wait_ge(s, v))
            self.n_wait += 1

    def op(self, eng, fn, reads=(), writes=(), inc=True):
        toks = []
        self._need(toks, reads, writes)
        sid = id(self.sem[eng])
        cur = self.cnt[eng] + 1
        toks = [t for t in toks if not (t[0] == sid and t[1] >= cur)]
        self._emit_waits(eng, toks)
        if inc:
            s = self.sem[eng]
            self.q[eng].append(lambda e, fn=fn, s=s: fn(e).then_inc(s, 1))
            self.cnt[eng] = cur
        else:
            self.q[eng].append(lambda e, fn=fn: fn(e))
        self._update((sid, cur), reads, writes)
        self.n_inst += 1

    def dma(self, eng, out, in_, **kw):
        reads = [in_.key] if in_.key is not None else []
        writes = [out.key] if out.key is not None else []
        toks = []
        self._need(toks, reads, writes)
        j = self.drr
        self.drr = (self.drr + 1) % len(self.dsem)
        s = self.dsem[j]
        sid = id(s)
        if self.dcnt[j] > 0:
            toks.append((sid, self.dcnt[j]))
        self._emit_waits(eng, toks)
        self.dcnt[j] += 16
        tok = (sid, self.dcnt[j])
        self.q[eng].append(lambda e, o=out.ap, i=in_.ap, s=s, kw=kw: e.dma_start(out=o, in_=i, **kw).then_inc(s, 16))
        self._update(tok, reads, writes)
        self.n_inst += 1
        return tok

    def barrier(self):
        toks = [(id(self.sem[e]), self.cnt[e]) for e in ENGS if self.cnt[e] > 0]
        toks += [(id(self.dsem[j]), self.dcnt[j]) for j in range(len(self.dsem)) if self.dcnt[j] > 0]
        for e in ENGS:
            own = id(self.sem[e])
            self._emit_waits(e, [t for t in toks if t[0] != own])

    def finish(self, final_toks):
        nc = self.nc
        self._emit_waits('sp', list(final_toks))
        q = self.q
        with nc.Block() as block:
            @block.tensor
            def _(e):
                for f in q['pe']:
                    f(e)

            @block.scalar
            def _(e):
                for f in q['act']:
                    f(e)

            @block.vector
            def _(e):
                for f in q['dve']:
                    f(e)

            @block.gpsimd
            def _(e):
                for f in q['pool']:
                    f(e)

            @block.sync
            def _(e):
                for f in q['sp']:
                    f(e)

    @staticmethod
    def _keys(*vs):
        out = []
        for v in vs:
            if isinstance(v, V) and v.key is not None:
                if isinstance(v.key, list):
                    out.extend(v.key)
                else:
                    out.append(v.key)
        return out

    @staticmethod
    def _a(v):
        return v.ap if isinstance(v, V) else v

    def mm(self, out, lhsT, rhs, start=True, stop=True, inc=True):
        self.op('pe', lambda e: e.matmul(out.ap, lhsT.ap, rhs.ap, start=start, stop=stop),
                reads=self._keys(lhsT, rhs), writes=self._keys(out), inc=inc)

    def tr(self, out, in_, ident, inc=True):
        self.op('pe', lambda e: e.transpose(out.ap, in_.ap, ident.ap),
                reads=self._keys(in_, ident), writes=self._keys(out), inc=inc)

    def act(self, out, in_, func, bias=None, scale=None, accum=None):
        kw = {}
        if bias is not None:
            kw['bias'] = self._a(bias)
        if scale is not None:
            kw['scale'] = self._a(scale)
        if accum is not None:
            kw['accum_out'] = accum.ap
        self.op('act', lambda e: e.activation(out.ap, in_.ap, func, **kw),
                reads=self._keys(in_, bias, scale), writes=self._keys(out, accum))

    def tt(self, eng, out, in0, in1, op):
        self.op(eng, lambda e: e.tensor_tensor(out.ap, in0.ap, in1.ap, op),
                reads=self._keys(in0, in1), writes=self._keys(out))

    def ts(self, eng, out, in0, s1, s2, op0, op1=None):
        a1, a2 = self._a(s1), self._a(s2)
        if op1 is None:
            self.op(eng, lambda e: e.tensor_scalar(out.ap, in0.ap, a1, None, op0),
                    reads=self._keys(in0, s1), writes=self._keys(out))
        else:
            self.op(eng, lambda e: e.tensor_scalar(out.ap, in0.ap, a1, a2, op0, op1),
                    reads=self._keys(in0, s1, s2), writes=self._keys(out))

    def stt(self, out, in0, scalar, in1, op0, op1):
        a = self._a(scalar)
        self.op('dve', lambda e: e.scalar_tensor_tensor(out.ap, in0.ap, a, in1.ap, op0, op1),
                reads=self._keys(in0, scalar, in1), writes=self._keys(out))

    def copy(self, eng, out, in_):
        if eng == 'act':
            self.op('act', lambda e: e.copy(out.ap, in_.ap), reads=self._keys(in_), writes=self._keys(out))
        else:
            self.op(eng, lambda e: e.tensor_copy(out.ap, in_.ap), reads=self._keys(in_), writes=self._keys(out))

    def memset(self, eng, out, val):
        self.op(eng, lambda e: e.memset(out.ap, val), writes=self._keys(out))

    def scan(self, out, d0, d1, init):
        a = self._a(init)
        self.op('dve', lambda e: e.tensor_tensor_scan(out.ap, d0.ap, d1.ap, a, ALU.mult, ALU.add),
                reads=self._keys(d0, d1, init), writes=self._keys(out))

    def max8(self, out, in_):
        self.op('dve', lambda e: e.max(out.ap, in_.ap), reads=self._keys(in_), writes=self._keys(out))

    def recip(self, out, in_):
        self.op('dve', lambda e: e.reciprocal(out.ap, in_.ap), reads=self._keys(in_), writes=self._keys(out))

    def reduce(self, out, in_, op, axis):
        self.op('dve', lambda e: e.tensor_reduce(out.ap, in_.ap, axis, op), reads=self._keys(in_), writes=self._keys(out))

    def bn_stats(self, out, in_):
        self.op('dve', lambda e: e.bn_stats(out.ap, in_.ap), reads=self._keys(in_), writes=self._keys(out))

    def bn_aggr(self, out, in_):
        self.op('dve', lambda e: e.bn_aggr(out.ap, in_.ap), reads=self._keys(in_), writes=self._keys(out))


class Arena:
    def __init__(self, nc, es, nwords):
        self.t = es.enter_context(nc.sbuf_tensor('arena', [128, nwords], F32))
        self.nwords = nwords
        self.off = 0
        self.uid = 0

    def reset(self):
        self.off = 0

    def alloc(self, name, shape, dt, parts=128):
        n = 1
        for s_ in shape:
            n *= s_
        nb = n * (2 if dt == BF16 else 4)
        nw = (nb + 3) // 4
        nw = (nw + 7) // 8 * 8
        assert self.off + nw <= self.nwords, ('arena overflow', name, self.off, nw, self.nwords)
        ap = self.t[0:parts, self.off:self.off + nw]
        self.off += nw
        if dt != F32:
            ap = ap.bitcast(dt)
        ap = ap[:, 0:n]
        if len(shape) == 2:
            ap = ap.rearrange('p (a b) -> p a b', a=shape[0])
        elif len(shape) == 3:
            ap = ap.rearrange('p (a b c) -> p a b c', a=shape[0], b=shape[1])
        self.uid += 1
        return TT('%s#%d' % (name, self.uid), ap)


class LoadCast:
    def __init__(self, P, stages, engs=('pool', 'dve', 'act')):
        self.P, self.stages, self.engs, self.i = P, stages, engs, 0

    def go(self, dst, src, n):
        st = self.stages[self.i % len(self.stages)]
        eng = self.engs[self.i % len(self.engs)]
        self.i += 1
        stv = V(st.ap[:, 0:n], st.key)
        self.P.dma('sp', stv, src)
        self.P.copy(eng, dst, stv)


def dram(ap, name, slot=None):
    return V(ap, (name, slot))


class Ctx:
    pass


def build_program(n_layers=L, stages='ABC', debug=False):
    nc = bass.Bass("TRN2", target_bir_lowering=False, dynamic_dma_scratch_size=8192)
    G = Ctx()
    ein = lambda n, shp: nc.dram_tensor(n, shp, F32, kind="ExternalInput").ap()
    G.x = ein("x", [S, D])
    G.memT = ein("memT", [D, NMEM])
    G.w_in = ein("w_in", [L, D, 5120])
    G.w_out = ein("w_out", [L, 1536, D])
    G.wq = ein("wq", [L, D, D])
    G.wk = ein("wk", [L, D, D])
    G.wv = ein("wv", [L, D, D])
    G.wo = ein("wo", [L, D, D])
    G.bd_a = ein("bd_a", [L, 4, 128, 128])
    G.bd_x = ein("bd_x", [L, 4, 128, 128])
    G.pp = ein("pp", [L, 128, NPP])
    G.bcp = ein("bcp", [L, 1, 512 + 4 * D])
    G.ident = ein("ident", [128, 128])
    G.E = ein("E", [32, S])
    G.trib = ein("trib", [2, 128, 256])
    G.vb = ein("vb", [2, 1, 64 * 32])
    G.out = nc.dram_tensor("out", [S, D], F32, kind="ExternalOutput").ap()
    kw = dict(kind="ExternalOutput") if debug else {}
    G.qT_d = nc.dram_tensor("qT_d", [512, S], BF16, **kw).ap()
    G.kT_d = nc.dram_tensor("kT_d", [512, S], BF16, **kw).ap()
    G.V_d = nc.dram_tensor("V_d", [S, 512], BF16, **kw).ap()
    G.gatt_d = nc.dram_tensor("gatt_d", [S, 512], F32, **kw).ap()
    G.zT_d = nc.dram_tensor("zT_d", [1024, S], BF16, **kw).ap()
    G.yatt_d = nc.dram_tensor("yatt_d", [S, 512], F32, **kw).ap()
    G.xmid_d = nc.dram_tensor("xmid_d", [S, D], F32, **kw).ap()
    G.km_d = nc.dram_tensor("km_d", [512, 32], F32, **kw).ap()

    with ExitStack() as es:
        P = Prog(nc, es)
        AR = Arena(nc, es, 45 * 1024)
        PB = []
        for i in range(8):
            t = es.enter_context(nc.psum_tensor('pb%d' % i, [128, 512], F32))
            PB.append(TT('pb%d' % i, t[:, :]))
        G.P, G.AR, G.PB, G.nc = P, AR, PB, nc
        final = []
        for l in range(n_layers):
            xin = G.x if l == 0 else G.xmid_d
            xin_name = 'x_in' if l == 0 else 'xmid'
            xout = G.xmid_d if l == 0 and n_layers > 1 else G.out
            xout_name = 'xmid' if l == 0 and n_layers > 1 else 'xout'
            if 'A' in stages:
                stage_A(G, l, xin, xin_name)
                P.barrier()
                AR.reset()
            if 'B' in stages:
                stage_B(G, l)
                P.barrier()
                AR.reset()
            if 'C' in stages:
                stage_C(G, l, xin, xin_name, xout, xout_name)
                P.barrier()
                AR.reset()
        final = [(id(P.dsem[j]), P.dcnt[j]) for j in range(len(P.dsem)) if P.dcnt[j] > 0]
        P.finish(final)
    return nc


def stage_A(G, l, xin, xin_name):
    P, AR, PB = G.P, G.AR, G.PB
    w_in = AR.alloc('w_in', [8, 5120], BF16)
    xs = AR.alloc('xs', [4, 1024], F32)
    xT = AR.alloc('xT', [8, 512], BF16)
    ident = AR.alloc('ident', [128], F32)
    ones = AR.alloc('ones', [128], BF16)
    bda = AR.alloc('bda', [4, 128], BF16)
    bdx = AR.alloc('bdx', [4, 128], BF16)
    pp = AR.alloc('pp', [NPP], F32)
    gaing = AR.alloc('gaing', [512], F32)
    cst = AR.alloc('cst', [8], F32)
    nsp8 = AR.alloc('nsp8', [4], F32)
    tmp4 = AR.alloc('tmp4', [4], F32)
    raw = AR.alloc('raw', [4, 516], F32)
    praw = AR.alloc('praw', [4, 516], F32)
    hcar = AR.alloc('hcar', [4], F32)
    kms = AR.alloc('kms', [4, 32], F32)
    xl2 = [AR.alloc('xl%d' % i, [512], F32) for i in range(2)]
    xlb2 = [AR.alloc('xlb%d' % i, [512], BF16) for i in range(2)]
    r_t = AR.alloc('r_t', [512], F32)
    i_t = AR.alloc('i_t', [512], F32)
    a_t = AR.alloc('a_t', [512], F32)
    q_t = AR.alloc('q_t', [512], F32)
    u_t = AR.alloc('u_t', [512], F32)
    yb = AR.alloc('yb', [4, 512], F32)
    sg = AR.alloc('sg', [4, 512], F32)
    scx = i_t
    sqb = AR.alloc('sqb', [2, 512], BF16)
    rt = AR.alloc('rt', [512], F32)
    rinv = AR.alloc('rinv', [512], F32)
    qo = AR.alloc('qo', [2, 512], BF16)
    ko = AR.alloc('ko', [2, 512], BF16)
    vo = AR.alloc('vo', [2, 512], BF16)
    go = AR.alloc('go', [2, 512], F32)
    zo = AR.alloc('zo', [2, 512], BF16)

    P.dma('sp', ident[:, :], dram(G.ident, 'c_ident'))
    P.dma('sp', pp[:, :], dram(G.pp[l], 'c_pp'))
    P.dma('sp', gaing[:, :], dram(bass.AP(G.bcp.tensor, l * (512 + 4 * D), [[0, 128], [1, 512]]), 'c_bcp'))
    stg = [yb.at(i)[:, i, :] for i in range(4)] + [sg.at(i)[:, i, :] for i in range(4)]
    lc = LoadCast(P, stg)
    for c in range(4):
        lc.go(bda[:, c, :], dram(G.bd_a[l, c], 'c_bda'), 128)
        lc.go(bdx[:, c, :], dram(G.bd_x[l, c], 'c_bdx'), 128)
    P.memset('pool', ones[:, :], 1.0)
    P.memset('pool', cst[:, 0:1], RMS_EPS)
    P.memset('pool', cst[:, 1:2], 1.0)
    P.memset('pool', raw[:, :, :], 0.0)
    P.memset('pool', praw[:, :, :], 0.0)
    P.memset('pool', hcar[:, :], 0.0)
    P.memset('pool', kms[:, :, :], 0.0)
    P.act(tmp4[:, :], pp[:, 28:32], AF.Exp, scale=-1.0)
    P.act(tmp4[:, :], tmp4[:, :], AF.Ln, bias=cst[:, 1:2])
    P.ts('dve', nsp8[:, :], tmp4[:, :], -8.0, None, ALU.mult)
    w_in_l = G.w_in[l].rearrange('(kc p) c -> p kc c', p=128)
    for s_ in (4, 0, 1, 2, 3, 5, 8, 7, 6, 9):
        for kc in range(8):
            lc.go(w_in.at(s_)[:, kc, s_ * 512:(s_ + 1) * 512],
                  dram(w_in_l[:, kc, s_ * 512:(s_ + 1) * 512], 'c_w_in'), 512)

    x_t = xin.rearrange('(t j p) d -> t p j d', j=4, p=128)

    def load_x(t):
        for j in range(4):
            P.dma('sp', xs.at(j)[:, j, :], dram(x_t[t][:, j, :], xin_name, t))

    bank_rr = [0]

    def nbank():
        b = PB[2 + bank_rr[0] % 6]
        bank_rr[0] += 1
        return b

    def fm(col0):
        b = nbank()
        s_ = col0 // 512
        for kc in range(8):
            P.mm(b[:, :], w_in.at(s_)[:, kc, col0:col0 + 128], xT[:, kc, :],
                 start=(kc == 0), stop=(kc == 7), inc=(kc == 7))
        return b

    def tm(col0, j):
        b = nbank()
        s_ = col0 // 512
        for kc in range(8):
            P.mm(b[:, :], xT[:, kc, j * 128:(j + 1) * 128], w_in.at(s_)[:, kc, col0:col0 + 512],
                 start=(kc == 0), stop=(kc == 7), inc=(kc == 7))
        return b

    def do_transposes(t):
        for kc in range(8):
            b = PB[kc % 2]
            for j in range(4):
                P.tr(b[:, j * 128:(j + 1) * 128], xs.at(j)[:, j, kc * 128:(kc + 1) * 128], ident[:, :], inc=(j == 3))
            if kc % 2 == 0:
                P.copy('act', xT[:, kc, :], b[:, :])
            else:
                P.copy('dve', xT[:, kc, :], b[:, :])
        if t + 1 < NT:
            load_x(t + 1)

    load_x(0)
    do_transposes(0)
    for t in range(NT):
        tok0 = t * 512
        def do_q(c):
            b = fm(0 + c * 128)
            P.ts('dve', qo.at(c % 2)[:, c % 2, :], b[:, :], 0.125, None, ALU.mult)
            P.dma('sp', dram(G.qT_d[c * 128:(c + 1) * 128, tok0:tok0 + 512], 'qT_d', t), qo.at(c % 2)[:, c % 2, :])

        def do_k(c):
            b = fm(512 + c * 128)
            for hb in range(2):
                P.act(ko.at(c % 2)[:, c % 2, hb * 256:(hb + 1) * 256], b[:, hb * 256:(hb + 1) * 256], AF.Copy,
                      accum=kms[:, c, 2 * t + hb:2 * t + hb + 1])
            P.dma('sp', dram(G.kT_d[c * 128:(c + 1) * 128, tok0:tok0 + 512], 'kT_d', t), ko.at(c % 2)[:, c % 2, :])

        def do_v(j):
            b = tm(1024, j)
            P.copy('dve', vo.at(j % 2)[:, j % 2, :], b[:, :])
            P.dma('sp', dram(G.V_d[tok0 + j * 128:tok0 + (j + 1) * 128, :], 'V_d', t), vo.at(j % 2)[:, j % 2, :])

        def do_g(j):
            b = tm(1536, j)
            P.act(go.at(j % 2)[:, j % 2, :], b[:, :], AF.Silu)
            P.tt('pool', go.at(j % 2)[:, j % 2, :], go.at(j % 2)[:, j % 2, :], gaing[:, :], ALU.mult)
            P.dma('sp', dram(G.gatt_d[tok0 + j * 128:tok0 + (j + 1) * 128, :], 'gatt_d', t), go.at(j % 2)[:, j % 2, :])

        def lru_front(c):
            xl_c = xl2[c % 2]
            b = fm(2048 + c * 128)
            P.copy('dve', raw.at(c)[:, c, 3:515], b[:, :])
            P.ts('dve', xl_c[:, :], raw.at(c)[:, c, 3:515], pp[:, c * 4 + 3:c * 4 + 4], pp[:, 16 + c:17 + c], ALU.mult, ALU.add)
            for k in range(3):
                P.stt(xl_c[:, :], raw.at(c)[:, c, k:k + 512], pp[:, c * 4 + k:c * 4 + k + 1], xl_c[:, :], ALU.mult, ALU.add)
            P.copy('pool', raw.at(c)[:, c, 0:3], raw.at(c)[:, c, 512:515])
            P.copy('pool', xlb2[c % 2][:, :], xl_c[:, :])

        def lru_chain(c):
            xl_c = xl2[c % 2]
            xlb_c = xlb2[c % 2]
            bg = nbank()
            P.mm(bg[:, :], bda[:, c, :], xlb_c[:, :])
            P.act(r_t[:, :], bg[:, :], AF.Sigmoid, bias=pp[:, 20 + c:21 + c])
            bg2 = nbank()
            P.mm(bg2[:, :], bdx[:, c, :], xlb_c[:, :])
            P.act(i_t[:, :], bg2[:, :], AF.Sigmoid, bias=pp[:, 24 + c:25 + c])
            P.act(a_t[:, :], r_t[:, :], AF.Exp, scale=nsp8[:, c:c + 1])
            P.act(q_t[:, :], a_t[:, :], AF.Square)
            P.act(q_t[:, :], q_t[:, :], AF.Sqrt, bias=cst[:, 1:2], scale=-1.0)
            P.tt('pool', u_t[:, :], i_t[:, :], xl_c[:, :], ALU.mult)
            P.tt('dve', u_t[:, :], u_t[:, :], q_t[:, :], ALU.mult)
            P.scan(yb.at(c)[:, c, :], a_t[:, :], u_t[:, :], hcar.at(c)[:, c:c + 1])
            P.copy('pool', hcar.at(c)[:, c:c + 1], yb.at(c)[:, c, 511:512])

        for c in range(4):
            lru_front(c)
            do_q(c)
            do_k(c)
            do_v(c)
            if c >= 1:
                lru_chain(c - 1)
            do_g(c)
        lru_chain(3)

        def norm1a(gate_col0):
            for c in range(4):
                b = fm(gate_col0 + c * 128)
                P.act(sg.at(c)[:, c, :], b[:, :], AF.Silu)

        def norm1(gate_col0):
            norm1a(gate_col0)
            norm1b()

        def norm1b():
            bs = nbank()
            for c in range(4):
                P.tt('pool', sqb.at(c % 2)[:, c % 2, :], yb.at(c)[:, c, :], yb.at(c)[:, c, :], ALU.mult)
                P.mm(bs[:, :], ones[:, :], sqb.at(c % 2)[:, c % 2, :], start=(c == 0), stop=(c == 3), inc=True)
            P.act(rt[:, :], bs[:, :], AF.Sqrt, bias=cst[:, 0:1], scale=1.0 / 512.0)
            P.recip(rinv[:, :], rt[:, :])

        def norm2(grp, gain_col):
            for c in range(4):
                P.stt(yb.at(c)[:, c, :], yb.at(c)[:, c, :], pp[:, gain_col + c:gain_col + c + 1], rinv[:, :], ALU.mult, ALU.mult)
                P.tt('pool', zo.at(c % 2)[:, c % 2, :], yb.at(c)[:, c, :], sg.at(c)[:, c, :], ALU.mult)
                r0 = (grp - 1) * 512 + c * 128
                P.dma('sp', dram(G.zT_d[r0:r0 + 128, tok0:tok0 + 512], 'zT_d', (grp, t)), zo.at(c % 2)[:, c % 2, :])

        def sc_front(c):
            bx = fm(4096 + c * 128)
            P.copy('dve', scx[:, :], bx[:, :])
            bc_ = fm(3584 + c * 128)
            P.tt('dve', praw.at(c)[:, c, 2:514], bc_[:, :], scx[:, :], ALU.mult)
            xc = xl2[c % 2]
            P.ts('dve', xc[:, :], praw.at(c)[:, c, 2:514], pp[:, 32 + c * 3 + 2:32 + c * 3 + 3], None, ALU.mult)
            for k in range(2):
                P.stt(xc[:, :], praw.at(c)[:, c, k:k + 512], pp[:, 32 + c * 3 + k:32 + c * 3 + k + 1], xc[:, :], ALU.mult, ALU.add)
            P.copy('pool', praw.at(c)[:, c, 0:2], praw.at(c)[:, c, 512:514])

        def sc_back(c):
            bb = fm(3072 + c * 128)
            P.tt('dve', yb.at(c)[:, c, :], bb[:, :], xl2[c % 2][:, :], ALU.mult)

        norm1(2560)
        sc_front(0)
        norm2(1, 44)
        sc_back(0)
        for c in range(1, 4):
            sc_front(c)
            sc_back(c)
        norm1a(4608)
        if t + 1 < NT:
            do_transposes(t + 1)
        norm1b()
        norm2(2, 48)
    for c in range(4):
        P.dma('sp', dram(G.km_d[c * 128:(c + 1) * 128, :], 'km_d'), kms[:, c, :])


def group_norm_out(G, l, t, grp, fm, gate_col0, yb, sg, sqb, rt, rinv, zo, cst, ones, pp, gain_col, nbank):
    P = G.P
    tok0 = t * 512
    for c in range(4):
        b = fm(gate_col0 + c * 128)
        P.act(sg.at(c)[:, c, :], b[:, :], AF.Silu)
    bs = nbank()
    for c in range(4):
        P.act(sqb.at(c % 2)[:, c % 2, :], yb.at(c)[:, c, :], AF.Square)
        P.mm(bs[:, :], ones[:, :], sqb.at(c % 2)[:, c % 2, :], start=(c == 0), stop=(c == 3), inc=True)
    P.act(rt[:, :], bs[:, :], AF.Sqrt, bias=cst[:, 0:1], scale=1.0 / 512.0)
    P.recip(rinv[:, :], rt[:, :])
    for c in range(4):
        P.stt(yb.at(c)[:, c, :], yb.at(c)[:, c, :], pp[:, gain_col + c:gain_col + c + 1], rinv[:, :], ALU.mult, ALU.mult)
        P.tt('pool', zo.at(c % 2)[:, c % 2, :], yb.at(c)[:, c, :], sg.at(c)[:, c, :], ALU.mult)
        r0 = (grp - 1) * 512 + c * 128
        P.dma('sp', dram(G.zT_d[r0:r0 + 128, tok0:tok0 + 512], 'zT_d', (grp, t)), zo.at(c % 2)[:, c % 2, :])


def stage_B(G, l):
    P, AR, PB = G.P, G.AR, G.PB
    ident = AR.alloc('identB', [128], F32)
    identb = AR.alloc('identb', [128], BF16)
    trib = AR.alloc('trib', [2, 256], BF16)
    vb1 = AR.alloc('vb1', [2048], F32)
    vb2 = AR.alloc('vb2', [2048], F32)
    kmf = AR.alloc('kmf', [4, 32], F32)
    kmb = AR.alloc('kmb', [8, 32], BF16)
    stg = AR.alloc('stgB', [2048], F32)
    qa = [AR.alloc('qa%d' % i, [S], BF16) for i in range(2)]
    ka = [AR.alloc('ka%d' % i, [S], BF16) for i in range(2)]
    va = [AR.alloc('va%d' % i, [64, 65], BF16) for i in range(2)]
    gm = AR.alloc('gm', [16, 32], F32)
    gm2 = AR.alloc('gm2', [16, 32], F32)
    top8 = AR.alloc('top8', [16, 8], F32)
    mbp = AR.alloc('mbp', [16, 96], F32)
    pT = [AR.alloc('pT%d' % i, [512], BF16) for i in range(4)]
    yo = AR.alloc('yo', [2, 4, 64], F32)
    rec = AR.alloc('rec', [2, 4], F32)

    P.dma('sp', ident[:, :], dram(G.ident, 'c_ident'))
    P.copy('dve', identb[:, :], ident[:, :])
    for r in range(2):
        P.dma('sp', stg[:, 0:256], dram(G.trib[r], 'c_trib'))
        P.copy('dve', trib[:, r, :], stg[:, 0:256])
    P.dma('sp', vb1[:, :], dram(bass.AP(G.vb.tensor, 0, [[0, 128], [1, 2048]]), 'c_vb'))
    P.dma('sp', vb2[:, :], dram(bass.AP(G.vb.tensor, 2048, [[0, 128], [1, 2048]]), 'c_vb'))
    for c in range(4):
        P.dma('sp', kmf[:, c, :], dram(G.km_d[c * 128:(c + 1) * 128, :], 'km_d'))
    P.memset('pool', mbp[:, :, :], 0.0)
    for i in range(2):
        P.memset('pool', va[i].at('one')[:, :, 64:65], 1.0)
        for c4 in range(4):
            sv = V(stg.ap[64:96, :], stg[:, :].key)
            P.dma('sp', sv, dram(G.E[:, c4 * 2048:(c4 + 1) * 2048], 'c_E'))
            P.copy('dve', V(ka[i].ap[64:96, c4 * 2048:(c4 + 1) * 2048], (ka[i].name, 'E')), sv)
    for h in range(8):
        par = h % 2
        P.copy('dve', V(kmb.ap[0:64, h, :], (kmb.name, h)), V(kmf.ap[64 * par:64 * par + 64, h // 2, :], (kmf.name, None)))

    v_r = G.V_d.rearrange('(t p) c -> p t c', p=128)
    y_r = G.yatt_d.rearrange('(t j p) c -> t p j c', j=4, p=128)

    def load_head(h):
        i = h % 2
        P.dma('sp', V(qa[i].ap[0:64, :], (qa[i].name, 'q')), dram(G.qT_d[64 * h:64 * h + 64, :], 'qT_d'))
        P.dma('sp', V(ka[i].ap[0:64, :], (ka[i].name, 'k')), dram(G.kT_d[64 * h:64 * h + 64, :], 'kT_d'))
        for g in range(4):
            P.dma('sp', V(va[i].ap[:, g * 16:(g + 1) * 16, 0:64], (va[i].name, 'v')),
                  dram(v_r[:, g * 16:(g + 1) * 16, 64 * h:64 * h + 64], 'V_d'))

    def gate_A(h, g16):
        i = h % 2
        if True:
            for s16 in range(16):
                sub = g16 * 16 + s16
                P.mm(PB[6][:, s16 * 32:(s16 + 1) * 32],
                     V(qa[i].ap[0:64, sub * 128:(sub + 1) * 128], (qa[i].name, 'q')),
                     V(kmb.ap[0:64, h, :], (kmb.name, h)), start=True, stop=True, inc=(s16 == 15))
            gmf = V(gm.ap[:, :, :].rearrange('p a b -> p (a b)'), gm[:, :, :].key)
            gm2f = V(gm2.ap[:, :, :].rearrange('p a b -> p (a b)'), gm2[:, :, :].key)
            P.tt('dve', gmf, PB[6][:, :], vb1[:, g16 * 512:(g16 + 1) * 512], ALU.add)
            P.tt('dve', gm2f, PB[6][:, :], vb2[:, g16 * 512:(g16 + 1) * 512], ALU.add)
            for s16 in range(16):
                P.max8(top8.at(s16)[:, s16, :], gm[:, s16, :])
            for s16 in range(16):
                P.ts('dve', mbp.at(s16)[:, s16, 64:96], gm2[:, s16, :], top8.at(s16)[:, s16, 2:3], -BIG, ALU.is_lt, ALU.mult)
    def gate_B(h, g16, dve_only=False):
        i = h % 2
        if True:
            for g4 in range(4):
                for s4 in range(4):
                    s16 = g4 * 4 + s4
                    P.tr(V(PB[7].ap[0:96, s4 * 128:(s4 + 1) * 128], PB[7][:, :].key), mbp.at(s16)[:, s16, 0:96], ident[:, :], inc=(s4 == 3))
                sub0 = g16 * 16 + g4 * 4
                eng = 'act' if (g4 % 2 == 0 and not dve_only) else 'dve'
                P.copy(eng, V(qa[i].ap[64:96, sub0 * 128:sub0 * 128 + 512], (qa[i].name, 'm')),
                       V(PB[7].ap[64:96, :], PB[7][:, :].key))

    def gate_phase(h):
        for g16 in range(4):
            gate_A(h, g16)
            gate_B(h, g16)

    def main_phase(h, hooks=None):
        i = h % 2
        qkeys = [(qa[i].name, 'q'), (qa[i].name, 'm')]
        kkeys = [(ka[i].name, 'k'), (ka[i].name, 'E')]
        vkeys = [(va[i].name, 'v'), (va[i].name, 'one')]
        units = []
        for qt in range(NT):
            for kt in range(4 * (qt + 1)):
                units.append((qt, kt))
        N = len(units)
        DEPTH = 3

        def emit_S(n):
            qt, kt = units[n]
            i_d = kt - 4 * qt
            c0 = 128 * i_d if i_d > 0 else 0
            sb = PB[2 + n % 4]
            P.mm(sb[:, c0:512], V(ka[i].ap[0:96, kt * 128:(kt + 1) * 128], kkeys),
                 V(qa[i].ap[0:96, qt * 512 + c0:(qt + 1) * 512], qkeys), start=True, stop=(i_d < 0), inc=(i_d < 0))
            if i_d >= 0:
                m, r = i_d // 2, i_d % 2
                lo, hi = max(c0, 256 * m), 256 * m + 256
                P.mm(sb[:, lo:hi], identb[:, :], trib[:, r, lo - 256 * m:256], start=False, stop=True, inc=True)
            P.act(pT[n % 4][:, c0:512], sb[:, c0:512], AF.Exp)

        def emit_PV(n):
            qt, kt = units[n]
            i_d = kt - 4 * qt
            c0 = 128 * i_d if i_d > 0 else 0
            acc = PB[qt % 2]
            last_kt = (kt == 4 * (qt + 1) - 1)
            for j in range(4):
                if 128 * j < c0:
                    continue
                P.mm(acc[:, j * 128:j * 128 + 65], pT[n % 4][:, j * 128:(j + 1) * 128],
                     V(va[i].ap[:, kt, 0:65], vkeys), start=(kt == 0 and j == 0), stop=(last_kt and j == 3),
                     inc=(j == 3))
            if last_kt:
                sl = qt % 2
                accv = acc.ap[:, :].rearrange('p (j c) -> p j c', j=4)
                P.recip(V(rec.ap[:, sl, :].unsqueeze(2), (rec.name, sl)), V(accv[:, :, 64:65], acc[:, :].key))
                P.tt('dve', yo.at(sl)[:, sl, :, :], V(accv[:, :, 0:64], acc[:, :].key),
                     V(rec.ap[:, sl, :].unsqueeze(2).broadcast_to([128, 4, 64]), (rec.name, sl)), ALU.mult)
                P.dma('sp', dram(y_r[qt][:, :, 64 * h:64 * h + 64], 'yatt_d', (h, qt)), yo.at(sl)[:, sl, :, :])

        for n in range(N + DEPTH):
            if hooks and n in hooks:
                hooks[n]()
            if n < N:
                emit_S(n)
            if n - DEPTH >= 0:
                emit_PV(n - DEPTH)

    load_head(0)
    gate_phase(0)
    for h in range(NHEAD_RUN):
        hooks = None
        if h + 1 < NHEAD_RUN:
            load_head(h + 1)
            hooks = {}
            for g16 in range(4):
                hooks[30 + 120 * g16] = (lambda hh=h + 1, g=g16: gate_A(hh, g))
                hooks[95 + 120 * g16] = (lambda hh=h + 1, g=g16: gate_B(hh, g, dve_only=True))
        main_phase(h, hooks)


def alias(tt, shape, dt):
    ap = tt.ap
    nd = len(ap.shape)
    if nd == 3:
        ap = ap.rearrange('p a b -> p (a b)')
    elif nd == 4:
        ap = ap.rearrange('p a b c -> p (a b c)')
    if ap.dtype != dt:
        ap = ap.bitcast(dt)
    n = 1
    for s_ in shape:
        n *= s_
    ap = ap[:, 0:n]
    if len(shape) == 2:
        ap = ap.rearrange('p (a b) -> p a b', a=shape[0])
    elif len(shape) == 3:
        ap = ap.rearrange('p (a b c) -> p a b c', a=shape[0], b=shape[1])
    return TT(tt.name, ap)


def stage_C(G, l, xin, xin_name, xout, xout_name):
    P, AR, PB = G.P, G.AR, G.PB
    w_out = AR.alloc('w_out', [12, 1024], BF16)
    wq = AR.alloc('wq', [8, 1024], BF16)
    wo = AR.alloc('wo', [8, 1024], BF16)
    KmT = AR.alloc('KmT', [8, 256], BF16)
    Vm = AR.alloc('Vm', [2, 4, 258], BF16)
    ident = AR.alloc('identC', [128], F32)
    gbc = AR.alloc('gbc', [4, 1024], F32)
    cst = AR.alloc('cstC', [8], F32)
    xres = [AR.alloc('xres%d' % i, [4, 1024], F32) for i in range(2)]
    bufAB = AR.alloc('bufAB', [4, 1024], F32)
    zT = AR.alloc('zT', [12, 512], BF16)
    x1T = AR.alloc('x1T', [8, 512], BF16)
    qT = AR.alloc('qTc', [8, 512], BF16)
    pT = [[AR.alloc('pTc%d_%d' % (i, m), [512], BF16) for m in range(2)] for i in range(2)]
    ss = AR.alloc('ss', [4], F32)
    rt4 = AR.alloc('rt4', [4], F32)
    r4 = AR.alloc('r4', [4], F32)
    st = AR.alloc('st', [4, 2, 6], F32)
    mv = AR.alloc('mv', [4, 2], F32)
    sd = AR.alloc('sd', [4], F32)
    rstd = AR.alloc('rstd', [4], F32)
    nb_ = AR.alloc('nb', [4], F32)
    rc = AR.alloc('rc', [8], F32)
    oT = alias(x1T, [8, 512], BF16)
    junk = alias(qT, [512], F32)
    obuf = AR.alloc('obuf', [4, 1024], F32)
    wk_v = alias(bufAB, [8, 1024], BF16)
    wv_v = alias(xres[0], [8, 1024], BF16)
    memT = alias(x1T, [8, 256], BF16)

    P.dma('sp', ident[:, :], dram(G.ident, 'c_ident'))
    for i in range(4):
        P.dma('sp', gbc[:, i, :], dram(bass.AP(G.bcp.tensor, l * (512 + 4 * D) + 512 + i * D, [[0, 128], [1, D]]), 'c_bcp'))
    P.memset('pool', cst[:, 0:1], RMS_EPS)
    P.memset('pool', cst[:, 1:2], LN_EPS)
    P.memset('pool', Vm[:, :, :, 256:257], 1.0)
    stg = [V(xres[1].ap[:, j, h2 * 512:(h2 + 1) * 512], (xres[1].name, (j, h2))) for j in range(4) for h2 in range(2)]
    lc = LoadCast(P, stg)

    def load_w(dst, src2d, nk):
        src = src2d.rearrange('(kc p) c -> p kc c', p=128)
        for kc in range(nk):
            for h2 in range(2):
                lc.go(dst[:, kc, h2 * 512:(h2 + 1) * 512], dram(src[:, kc, h2 * 512:(h2 + 1) * 512], 'c_w'), 512)

    mt = G.memT.rearrange('(kc p) m -> p kc m', p=128)
    for kc in range(8):
        lc.go(memT[:, kc, :], dram(mt[:, kc, :], 'c_mem'), 256)
    load_w(wk_v, G.wk[l], 8)
    load_w(wv_v, G.wv[l], 8)
    rr = [0]

    def nbank():
        b = PB[2 + rr[0] % 6]
        rr[0] += 1
        return b

    ev = [0]

    def evac(out, in_, scale=None):
        ev[0] += 1
        if scale is not None:
            P.act(out, in_, AF.Copy, scale=scale)
        elif ev[0] % 2 == 0:
            P.copy('act', out, in_)
        else:
            P.copy('dve', out, in_)

    for oc in range(8):
        b = nbank()
        for kc in range(8):
            P.mm(b[:, 0:256], wk_v[:, kc, oc * 128:(oc + 1) * 128], memT[:, kc, :], start=(kc == 0), stop=(kc == 7), inc=(kc == 7))
        evac(KmT[:, oc, :], b[:, 0:256])
    for mh in range(2):
        for half in range(2):
            b = nbank()
            for kc in range(8):
                P.mm(b[:, :], memT[:, kc, mh * 128:(mh + 1) * 128], wv_v[:, kc, half * 512:(half + 1) * 512],
                     start=(kc == 0), stop=(kc == 7), inc=(kc == 7))
            evac(Vm[:, mh, 2 * half:2 * half + 2, 0:256], V(b.ap[:, :].rearrange('p (a b) -> p a b', a=2), b[:, :].key))
    load_w(w_out, G.w_out[l], 12)
    load_w(wq, G.wq[l], 8)
    load_w(wo, G.wo[l], 8)

    x_t = xin.rearrange('(t j p) d -> t p j d', j=4, p=128)
    o_t = xout.rearrange('(t j p) d -> t p j d', j=4, p=128)
    ya_t = G.yatt_d.rearrange('(t j p) c -> t p j c', j=4, p=128)
    ga_t = G.gatt_d.rearrange('(t j p) c -> t p j c', j=4, p=128)
    zT_r = G.zT_d.rearrange('(kc p) t -> p kc t', p=128)

    def ln_p1(xr, j):
        for h2 in range(2):
            P.bn_stats(st.at(j)[:, j, h2, :], xr.at(j)[:, j, h2 * 512:(h2 + 1) * 512])
        P.bn_aggr(mv.at(j)[:, j, :], V(st.ap[:, j, :, :].rearrange('p a b -> p (a b)'), (st.name, j)))
        P.act(sd.at(j)[:, j:j + 1], mv.at(j)[:, j, 1:2], AF.Sqrt, bias=cst[:, 1:2])

    def ln_p2(xr, j):
        P.recip(rstd.at(j)[:, j:j + 1], sd.at(j)[:, j:j + 1])
        P.ts('dve', nb_.at(j)[:, j:j + 1], mv.at(j)[:, j, 0:1], -1.0, rstd.at(j)[:, j:j + 1], ALU.mult, ALU.mult)
        P.act(xr.at(j)[:, j, :], xr.at(j)[:, j, :], AF.Identity, bias=nb_.at(j)[:, j:j + 1], scale=rstd.at(j)[:, j:j + 1])

    def ln_p3(xr, j, gi):
        P.tt('dve', xr.at(j)[:, j, :], xr.at(j)[:, j, :], gbc[:, gi, :], ALU.mult)
        P.tt('pool', xr.at(j)[:, j, 0:512], xr.at(j)[:, j, 0:512], gbc[:, gi + 1, 0:512], ALU.add)
        P.tt('dve', xr.at(j)[:, j, 512:1024], xr.at(j)[:, j, 512:1024], gbc[:, gi + 1, 512:1024], ALU.add)

    def ln_pipeline(xr, gi, mm_fn, after_fn, mid_fn=None, mid_early=False):
        if mid_early:
            sched = [('1', 0), ('1', 1), ('M', 0), ('2', 0), ('1', 2), ('2', 1), ('3', 0), ('1', 3), ('A', 0), ('2', 2),
                     ('3', 1), ('A', 1), ('2', 3), ('3', 2), ('A', 2), ('3', 3), ('A', 3)]
        else:
            sched = [('1', 0), ('1', 1), ('2', 0), ('1', 2), ('2', 1), ('3', 0), ('1', 3), ('M', 0), ('A', 0), ('2', 2),
                     ('3', 1), ('A', 1), ('2', 3), ('3', 2), ('A', 2), ('3', 3), ('A', 3)]
        for ph, j in sched:
            if ph == '1':
                mm_fn(j)
                ln_p1(xr, j)
            elif ph == '2':
                ln_p2(xr, j)
            elif ph == '3':
                ln_p3(xr, j, gi)
            elif ph == 'M':
                if mid_fn is not None:
                    mid_fn()
            else:
                after_fn(j)

    tr_rr = [0]

    def transposes_j(src_fn, dst, j, nkc):
        for g in range(nkc // 4):
            b = PB[tr_rr[0] % 2]
            tr_rr[0] += 1
            for k4 in range(4):
                P.tr(b[:, k4 * 128:(k4 + 1) * 128], src_fn(j, g * 4 + k4), ident[:, :], inc=(k4 == 3))
            evac(V(dst.ap[:, g * 4:(g + 1) * 4, j * 128:(j + 1) * 128], dst[:, :, :].key),
                 V(b.ap[:, :].rearrange('p (a b) -> p a b', a=4), b[:, :].key))

    def transposes(src_fn, dst, nkc):
        for kc in range(nkc):
            b = PB[kc % 2]
            for j in range(4):
                P.tr(b[:, j * 128:(j + 1) * 128], src_fn(j, kc), ident[:, :], inc=(j == 3))
            evac(dst[:, kc, :], b[:, :])

    def load_tile(T):
        xr = xres[T % 2]
        for j in range(4):
            P.dma('sp', xr.at(j)[:, j, :], dram(x_t[T][:, j, :], xin_name, T))
        P.dma('sp', bufAB[:, :, 0:512], dram(ya_t[T], 'yatt_d'))
        P.dma('sp', bufAB[:, :, 512:1024], dram(ga_t[T], 'gatt_d'))

    def load_tile_b(T):
        P.dma('sp', zT[:, 4:12, :], dram(zT_r[:, :, T * 512:(T + 1) * 512], 'zT_d'))

    P.memset('pool', xres[1][:, 0, 0:1], 0.0)
    def att_norm():
        for j in range(4):
            P.act(junk[:, :], bufAB[:, j, 0:512], AF.Square, accum=ss.at(j)[:, j:j + 1])
        P.act(rt4[:, :], ss[:, :], AF.Sqrt, bias=cst[:, 0:1], scale=1.0 / 512.0)
        P.recip(r4[:, :], rt4[:, :])
        for j in range(4):
            P.stt(bufAB[:, j, 0:512], bufAB[:, j, 0:512], r4[:, j:j + 1], bufAB[:, j, 512:1024], ALU.mult, ALU.mult)

    def z_att_tr():
        transposes(lambda j, kc: bufAB[:, j, kc * 128:(kc + 1) * 128], zT, 4)

    load_tile(0)
    load_tile_b(0)
    att_norm()
    z_att_tr()
    for T in range(NT):
        xr = xres[T % 2]
        if T + 1 < NT:
            load_tile(T + 1)
        def mm_wout(j, xr=xr):
            for h2 in range(2):
                b = nbank()
                for kc in range(12):
                    P.mm(b[:, :], zT[:, kc, j * 128:(j + 1) * 128], w_out[:, kc, h2 * 512:(h2 + 1) * 512],
                         start=(kc == 0), stop=(kc == 11), inc=(kc == 11))
                P.stt(xr.at(j)[:, j, h2 * 512:(h2 + 1) * 512], xr.at(j)[:, j, h2 * 512:(h2 + 1) * 512], ALPHA, b[:, :], ALU.mult, ALU.add)

        ln_pipeline(xr, 0, mm_wout,
                    lambda j, xr=xr: transposes_j(lambda jj, kc: xr.at(jj)[:, jj, kc * 128:(kc + 1) * 128], x1T, j, 8),
                    mid_fn=(lambda T=T: load_tile_b(T + 1)) if T + 1 < NT else None)
        for oc in range(8):
            b = nbank()
            for kc in range(8):
                P.mm(b[:, :], wq[:, kc, oc * 128:(oc + 1) * 128], x1T[:, kc, :], start=(kc == 0), stop=(kc == 7), inc=(kc == 7))
            evac(qT[:, oc, :], b[:, :], scale=1.0 / 16.0)

        def emit_S(hd):
            pb_ = pT[hd % 2]
            for mh in range(2):
                b = nbank()
                for cc in range(2):
                    P.mm(b[:, :], KmT[:, 2 * hd + cc, mh * 128:(mh + 1) * 128], qT[:, 2 * hd + cc, :],
                         start=(cc == 0), stop=(cc == 1), inc=(cc == 1))
                P.act(pb_[mh][:, :], b[:, :], AF.Exp)

        def emit_PV(hd):
            pb_ = pT[hd % 2]
            for j in range(4):
                b = nbank()
                for mh in range(2):
                    P.mm(b[:, 0:257], pb_[mh][:, j * 128:(j + 1) * 128], Vm[:, mh, hd, 0:257],
                         start=(mh == 0), stop=(mh == 1), inc=(mh == 1))
                k8 = (hd * 4 + j) % 8
                P.recip(rc.at(k8)[:, k8:k8 + 1], b[:, 256:257])
                P.ts('dve', obuf[:, j, hd * 256:(hd + 1) * 256], b[:, 0:256], rc.at(k8)[:, k8:k8 + 1], None, ALU.mult)

        emit_S(0)
        for hd in range(4):
            if hd + 1 < 4:
                emit_S(hd + 1)
            emit_PV(hd)
        transposes(lambda j, kc: obuf[:, j, kc * 128:(kc + 1) * 128], oT, 8)
        if T + 1 < NT:
            att_norm()
        def mm_wo(j, xr=xr):
            for h2 in range(2):
                b = nbank()
                for kc in range(8):
                    P.mm(b[:, :], oT[:, kc, j * 128:(j + 1) * 128], wo[:, kc, h2 * 512:(h2 + 1) * 512],
                         start=(kc == 0), stop=(kc == 7), inc=(kc == 7))
                P.stt(xr.at(j)[:, j, h2 * 512:(h2 + 1) * 512], xr.at(j)[:, j, h2 * 512:(h2 + 1) * 512], ALPHA, b[:, :], ALU.mult, ALU.add)

        ln_pipeline(xr, 2, mm_wo,
                    lambda j, xr=xr, T=T: P.dma('sp', dram(o_t[T][:, j, :], xout_name, T), xr.at(j)[:, j, :]),
                    mid_fn=z_att_tr if T + 1 < NT else None, mid_early=True)


def host_constants():
    ident = np.eye(128, dtype=np.float32)
    E = np.zeros((32, S), np.float32)
    for j in range(32):
        E[j, j * 256:(j + 1) * 256] = 1.0
    trib = np.zeros((2, 128, 256), np.float32)
    kk = np.arange(128)[:, None]
    qq = np.arange(256)[None, :]
    trib[0] = np.where(qq >= kk, 0.0, -BIG)
    trib[1] = np.where(qq >= kk + 128, 0.0, -BIG)
    vb = np.zeros((2, 1, 64, 32), np.float32)
    for sub in range(64):
        own = sub // 2
        for blk in range(32):
            if blk < own:
                vb[0, 0, sub, blk] = 0.0
                vb[1, 0, sub, blk] = 0.0
            elif blk == own:
                vb[0, 0, sub, blk] = -BIG
                vb[1, 0, sub, blk] = BIG
            else:
                vb[0, 0, sub, blk] = -BIG
                vb[1, 0, sub, blk] = -2 * BIG
    return ident, E, trib, vb.reshape(2, 1, 64 * 32)


def host_layout(inputs):
    f = lambda n: np.ascontiguousarray(np.asarray(inputs[n], dtype=np.float32))
    lw_a, lw_x = f('lru_w_a'), f('lru_w_x')
    bd_a = np.zeros((L, 4, 128, 128), np.float32)
    bd_x = np.zeros((L, 4, 128, 128), np.float32)
    for l in range(L):
        for c in range(4):
            for hh in range(2):
                bd_a[l, c, hh * 64:(hh + 1) * 64, hh * 64:(hh + 1) * 64] = lw_a[l, 2 * c + hh]
                bd_x[l, c, hh * 64:(hh + 1) * 64, hh * 64:(hh + 1) * 64] = lw_x[l, 2 * c + hh]
    pp = np.zeros((L, 128, NPP), np.float32)
    lcw, lcb, b_a, b_x, lam = f('lru_conv_w'), f('lru_conv_b'), f('lru_b_a'), f('lru_b_x'), f('lru_lambda')
    scw, gg = f('sc_conv_w'), f('group_gain')
    for l in range(L):
        for c in range(4):
            sl = slice(c * 128, (c + 1) * 128)
            for k in range(4):
                pp[l, :, c * 4 + k] = lcw[l, k, sl]
            pp[l, :, 16 + c] = lcb[l, sl]
            pp[l, :, 20 + c] = b_a[l, sl]
            pp[l, :, 24 + c] = b_x[l, sl]
            pp[l, :, 28 + c] = lam[l, sl]
            for k in range(3):
                pp[l, :, 32 + c * 3 + k] = scw[l, k, sl]
            pp[l, :, 44 + c] = gg[l, 1, sl]
            pp[l, :, 48 + c] = gg[l, 2, sl]
    bcp = np.concatenate([gg[:, 0, :], f('ln1_g'), f('ln1_b'), f('ln2_g'), f('ln2_b')], axis=1).reshape(L, 1, 512 + 4 * D)
    ident, E, trib, vb = host_constants()
    shared = dict(w_in=f('w_in'), w_out=f('w_out'), wq=f('xq_w'), wk=f('xk_w'), wv=f('xv_w'), wo=f('xo_w'),
                  bd_a=bd_a, bd_x=bd_x, pp=pp, bcp=np.ascontiguousarray(bcp), ident=ident, E=E, trib=trib, vb=vb)
    x, mem = f('x'), f('mem')
    maps = []
    for b in range(x.shape[0]):
        m = dict(shared)
        m['x'] = np.ascontiguousarray(x[b])
        m['memT'] = np.ascontiguousarray(mem[b].T)
        maps.append(m)
    return maps


_NC_CACHE = {}


def kernel(**inputs):
    maps = host_layout(inputs)
    if 'nc' not in _NC_CACHE:
        _NC_CACHE['nc'] = build_program()
    nc = _NC_CACHE['nc']
    B = len(maps)
    in_maps = [maps[i % B] for i in range(8)]
    res = run_bass_kernel_spmd(nc, in_maps, core_ids=list(range(8)))
    out = np.stack([np.asarray(res.results[b]['out'], dtype=np.float32) for b in range(B)], axis=0)
    return out
```

```python
from contextlib import ExitStack
import numpy as np
import concourse.bass as bass
import concourse.mybir as mybir
from concourse.bass_utils import run_bass_kernel_spmd

F32 = mybir.dt.float32
BF16 = mybir.dt.bfloat16
ALU = mybir.AluOpType
AF = mybir.ActivationFunctionType
AX = mybir.AxisListType

S = 8192
D = 1024
L = 2
NMEM = 256
NT = S // 512
ALPHA = float((2.0 * L) ** 0.25)
BIG = 1000.0
NPP = 52
STOP = None
NHEAD_RUN = 8
LN_EPS = 1e-5
RMS_EPS = 1e-6

ENGS = ['pe', 'act', 'dve', 'pool', 'sp']


class V:
    __slots__ = ('ap', 'key')

    def __init__(self, ap, key):
        self.ap = ap
        self.key = key


class _Slot:
    def __init__(self, t, slot):
        self.t = t
        self.slot = slot

    def __getitem__(self, idx):
        return V(self.t.ap[idx], (self.t.name, self.slot))


class TT:
    def __init__(self, name, ap):
        self.name = name
        self.ap = ap

    def __getitem__(self, idx):
        return V(self.ap[idx], (self.name, None))

    def at(self, slot):
        return _Slot(self, slot)


class Prog:
    def __init__(self, nc, es, n_dma_sems=32):
        self.nc = nc
        self.q = {e: [] for e in ENGS}
        self.sem = {e: es.enter_context(nc.semaphore('s_' + e)) for e in ENGS}
        self.cnt = {e: 0 for e in ENGS}
        self.dsem = [es.enter_context(nc.semaphore('d%d' % i)) for i in range(n_dma_sems)]
        self.dcnt = [0] * n_dma_sems
        self.drr = 0
        self.waited = {e: {} for e in ENGS}
        self.state = {}
        self.sems_by_id = {}
        self.n_wait = 0
        self.n_inst = 0
        for e in ENGS:
            self._semid(self.sem[e])
        for s in self.dsem:
            self._semid(s)

    def _semid(self, s):
        i = id(s)
        self.sems_by_id[i] = s
        return i

    def _slots(self, key):
        name, slot = key
        d = self.state.setdefault(name, {})
        if slot is None:
            if None not in d:
                d[None] = {'w': None, 'r': []}
            return list(d.values())
        if slot not in d:
            d[slot] = {'w': None, 'r': []}
        out = [d[slot]]
        if None in d:
            out.append(d[None])
        return out

    def _need(self, toks, reads, writes):
        for k in reads:
            for st in self._slots(k):
                if st['w'] is not None:
                    toks.append(st['w'])
        for k in writes:
            for st in self._slots(k):
                if st['w'] is not None:
                    toks.append(st['w'])
                toks.extend(st['r'])

    def _update(self, tok, reads, writes):
        for (name, slot) in reads:
            st = self.state[name][slot]
            st['r'] = [t for t in st['r'] if t[0] != tok[0]] + [tok]
        for (name, slot) in writes:
            d = self.state[name]
            if slot is None:
                for s in list(d.keys()):
                    d[s] = {'w': tok, 'r': []}
            else:
                d[slot] = {'w': tok, 'r': []}

    def _emit_waits(self, eng, toks):
        best = {}
        for (sid, v) in toks:
            if v > best.get(sid, 0):
                best[sid] = v
        w = self.waited[eng]
        for sid, v in best.items():
            if w.get(sid, 0) >= v:
                continue
            w[sid] = v
            s = self.sems_by_id[sid]
            self.q[eng].append(lambda e, s=s, v=v: e.wait_ge(s, v))
            self.n_wait += 1

    def op(self, eng, fn, reads=(), writes=(), inc=True):
        toks = []
        self._need(toks, reads, writes)
        sid = id(self.sem[eng])
        cur = self.cnt[eng] + 1
        toks = [t for t in toks if not (t[0] == sid and t[1] >= cur)]
        self._emit_waits(eng, toks)
        if inc:
            s = self.sem[eng]
            self.q[eng].append(lambda e, fn=fn, s=s: fn(e).then_inc(s, 1))
            self.cnt[eng] = cur
        else:
            self.q[eng].append(lambda e, fn=fn: fn(e))
        self._update((sid, cur), reads, writes)
        self.n_inst += 1

    def dma(self, eng, out, in_, **kw):
        reads = [in_.key] if in_.key is not None else []
        writes = [out.key] if out.key is not None else []
        toks = []
        self._need(toks, reads, writes)
        j = self.drr
        self.drr = (self.drr + 1) % len(self.dsem)
        s = self.dsem[j]
        sid = id(s)
        if self.dcnt[j] > 0:
            toks.append((sid, self.dcnt[j]))
        self._emit_waits(eng, toks)
        self.dcnt[j] += 16
        tok = (sid, self.dcnt[j])
        self.q[eng].append(lambda e, o=out.ap, i=in_.ap, s=s, kw=kw: e.dma_start(out=o, in_=i, **kw).then_inc(s, 16))
        self._update(tok, reads, writes)
        self.n_inst += 1
        return tok

    def barrier(self):
        toks = [(id(self.sem[e]), self.cnt[e]) for e in ENGS if self.cnt[e] > 0]
        toks += [(id(self.dsem[j]), self.dcnt[j]) for j in range(len(self.dsem)) if self.dcnt[j] > 0]
        for e in ENGS:
            own = id(self.sem[e])
            self._emit_waits(e, [t for t in toks if t[0] != own])

    def finish(self, final_toks):
        nc = self.nc
        self._emit_waits('sp', list(final_toks))
        q = self.q
        with nc.Block() as block:
            @block.tensor
            def _(e):
                for f in q['pe']:
                    f(e)

            @block.scalar
            def _(e):
                for f in q['act']:
                    f(e)

            @block.vector
            def _(e):
                for f in q['dve']:
                    f(e)

            @block.gpsimd
            def _(e):
                for f in q['pool']:
                    f(e)

            @block.sync
            def _(e):
                for f in q['sp']:
                    f(e)

    @staticmethod
    def _keys(*vs):
        out = []
        for v in vs:
            if isinstance(v, V) and v.key is not None:
                if isinstance(v.key, list):
                    out.extend(v.key)
                else:
                    out.append(v.key)
        return out

    @staticmethod
    def _a(v):
        return v.ap if isinstance(v, V) else v

    def mm(self, out, lhsT, rhs, start=True, stop=True, inc=True):
        self.op('pe', lambda e: e.matmul(out.ap, lhsT.ap, rhs.ap, start=start, stop=stop),
                reads=self._keys(lhsT, rhs), writes=self._keys(out), inc=inc)

    def tr(self, out, in_, ident, inc=True):
        self.op('pe', lambda e: e.transpose(out.ap, in_.ap, ident.ap),
                reads=self._keys(in_, ident), writes=self._keys(out), inc=inc)

    def act(self, out, in_, func, bias=None, scale=None, accum=None):
        kw = {}
        if bias is not None:
            kw['bias'] = self._a(bias)
        if scale is not None:
            kw['scale'] = self._a(scale)
        if accum is not None:
            kw['accum_out'] = accum.ap
        self.op('act', lambda e: e.activation(out.ap, in_.ap, func, **kw),
                reads=self._keys(in_, bias, scale), writes=self._keys(out, accum))

    def tt(self, eng, out, in0, in1, op):
        self.op(eng, lambda e: e.tensor_tensor(out.ap, in0.ap, in1.ap, op),
                reads=self._keys(in0, in1), writes=self._keys(out))

    def ts(self, eng, out, in0, s1, s2, op0, op1=None):
        a1, a2 = self._a(s1), self._a(s2)
        if op1 is None:
            self.op(eng, lambda e: e.tensor_scalar(out.ap, in0.ap, a1, None, op0),
                    reads=self._keys(in0, s1), writes=self._keys(out))
        else:
            self.op(eng, lambda e: e.tensor_scalar(out.ap, in0.ap, a1, a2, op0, op1),
                    reads=self._keys(in0, s1, s2), writes=self._keys(out))

    def stt(self, out, in0, scalar, in1, op0, op1):
        a = self._a(scalar)
        self.op('dve', lambda e: e.scalar_tensor_tensor(out.ap, in0.ap, a, in1.ap, op0, op1),
                reads=self._keys(in0, scalar, in1), writes=self._keys(out))

    def copy(self, eng, out, in_):
        if eng == 'act':
            self.op('act', lambda e: e.copy(out.ap, in_.ap), reads=self._keys(in_), writes=self._keys(out))
        else:
            self.op(eng, lambda e: e.tensor_copy(out.ap, in_.ap), reads=self._keys(in_), writes=self._keys(out))

    def memset(self, eng, out, val):
        self.op(eng, lambda e: e.memset(out.ap, val), writes=self._keys(out))

    def scan(self, out, d0, d1, init):
        a = self._a(init)
        self.op('dve', lambda e: e.tensor_tensor_scan(out.ap, d0.ap, d1.ap, a, ALU.mult, ALU.add),
                reads=self._keys(d0, d1, init), writes=self._keys(out))

    def max8(self, out, in_):
        self.op('dve', lambda e: e.max(out.ap, in_.ap), reads=self._keys(in_), writes=self._keys(out))

    def recip(self, out, in_):
        self.op('dve', lambda e: e.reciprocal(out.ap, in_.ap), reads=self._keys(in_), writes=self._keys(out))

    def reduce(self, out, in_, op, axis):
        self.op('dve', lambda e: e.tensor_reduce(out.ap, in_.ap, axis, op), reads=self._keys(in_), writes=self._keys(out))

    def bn_stats(self, out, in_):
        self.op('dve', lambda e: e.bn_stats(out.ap, in_.ap), reads=self._keys(in_), writes=self._keys(out))

    def bn_aggr(self, out, in_):
        self.op('dve', lambda e: e.bn_aggr(out.ap, in_.ap), reads=self._keys(in_), writes=self._keys(out))


class Arena:
    def __init__(self, nc, es, nwords):
        self.t = es.enter_context(nc.sbuf_tensor('arena', [128, nwords], F32))
        self.nwords = nwords
        self.off = 0
        self.uid = 0

    def reset(self):
        self.off = 0

    def alloc(self, name, shape, dt, parts=128):
        n = 1
        for s_ in shape:
            n *= s_
        nb = n * (2 if dt == BF16 else 4)
        nw = (nb + 3) // 4
        nw = (nw + 7) // 8 * 8
        assert self.off + nw <= self.nwords, ('arena overflow', name, self.off, nw, self.nwords)
        ap = self.t[0:parts, self.off:self.off + nw]
        self.off += nw
        if dt != F32:
            ap = ap.bitcast(dt)
        ap = ap[:, 0:n]
        if len(shape) == 2:
            ap = ap.rearrange('p (a b) -> p a b', a=shape[0])
        elif len(shape) == 3:
            ap = ap.rearrange('p (a b c) -> p a b c', a=shape[0], b=shape[1])
        self.uid += 1
        return TT('%s#%d' % (name, self.uid), ap)


class LoadCast:
    def __init__(self, P, stages, engs=('pool', 'dve', 'act')):
        self.P, self.stages, self.engs, self.i = P, stages, engs, 0

    def go(self, dst, src, n):
        st = self.stages[self.i % len(self.stages)]
        eng = self.engs[self.i % len(self.engs)]
        self.i += 1
        stv = V(st.ap[:, 0:n], st.key)
        self.P.dma('sp', stv, src)
        self.P.copy(eng, dst, stv)


def dram(ap, name, slot=None):
    return V(ap, (name, slot))


class Ctx:
    pass


def build_program(n_layers=L, stages='ABC', debug=False):
    nc = bass.Bass("TRN2", target_bir_lowering=False, dynamic_dma_scratch_size=8192)
    G = Ctx()
    ein = lambda n, shp: nc.dram_tensor(n, shp, F32, kind="ExternalInput").ap()
    G.x = ein("x", [S, D])
    G.memT = ein("memT", [D, NMEM])
    G.w_in = ein("w_in", [L, D, 5120])
    G.w_out = ein("w_out", [L, 1536, D])
    G.wq = ein("wq", [L, D, D])
    G.wk = ein("wk", [L, D, D])
    G.wv = ein("wv", [L, D, D])
    G.wo = ein("wo", [L, D, D])
    G.bd_a = ein("bd_a", [L, 4, 128, 128])
    G.bd_x = ein("bd_x", [L, 4, 128, 128])
    G.pp = ein("pp", [L, 128, NPP])
    G.bcp = ein("bcp", [L, 1, 512 + 4 * D])
    G.ident = ein("ident", [128, 128])
    G.E = ein("E", [32, S])
    G.trib = ein("trib", [2, 128, 256])
    G.vb = ein("vb", [2, 1, 64 * 32])
    G.out = nc.dram_tensor("out", [S, D], F32, kind="ExternalOutput").ap()
    kw = dict(kind="ExternalOutput") if debug else {}
    G.qT_d = nc.dram_tensor("qT_d", [512, S], BF16, **kw).ap()
    G.kT_d = nc.dram_tensor("kT_d", [512, S], BF16, **kw).ap()
    G.V_d = nc.dram_tensor("V_d", [S, 512], BF16, **kw).ap()
    G.gatt_d = nc.dram_tensor("gatt_d", [S, 512], F32, **kw).ap()
    G.zT_d = nc.dram_tensor("zT_d", [1024, S], BF16, **kw).ap()
    G.yatt_d = nc.dram_tensor("yatt_d", [S, 512], F32, **kw).ap()
    G.xmid_d = nc.dram_tensor("xmid_d", [S, D], F32, **kw).ap()
    G.km_d = nc.dram_tensor("km_d", [512, 32], F32, **kw).ap()

    with ExitStack() as es:
        P = Prog(nc, es)
        AR = Arena(nc, es, 45 * 1024)
        PB = []
        for i in range(8):
            t = es.enter_context(nc.psum_tensor('pb%d' % i, [128, 512], F32))
            PB.append(TT('pb%d' % i, t[:, :]))
        G.P, G.AR, G.PB, G.nc = P, AR, PB, nc
        final = []
        for l in range(n_layers):
            xin = G.x if l == 0 else G.xmid_d
            xin_name = 'x_in' if l == 0 else 'xmid'
            xout = G.xmid_d if l == 0 and n_layers > 1 else G.out
            xout_name = 'xmid' if l == 0 and n_layers > 1 else 'xout'
            if 'A' in stages:
                stage_A(G, l, xin, xin_name)
                P.barrier()
                AR.reset()
            if 'B' in stages:
                stage_B(G, l)
                P.barrier()
                AR.reset()
            if 'C' in stages:
                stage_C(G, l, xin, xin_name, xout, xout_name)
                P.barrier()
                AR.reset()
        final = [(id(P.dsem[j]), P.dcnt[j]) for j in range(len(P.dsem)) if P.dcnt[j] > 0]
        P.finish(final)
    return nc


def stage_A(G, l, xin, xin_name):
    P, AR, PB = G.P, G.AR, G.PB
    w_in = AR.alloc('w_in', [8, 5120], BF16)
    xs = AR.alloc('xs', [4, 1024], F32)
    xT = AR.alloc('xT', [8, 512], BF16)
    ident = AR.alloc('ident', [128], F32)
    ones = AR.alloc('ones', [128], BF16)
    bda = AR.alloc('bda', [4, 128], BF16)
    bdx = AR.alloc('bdx', [4, 128], BF16)
    pp = AR.alloc('pp', [NPP], F32)
    gaing = AR.alloc('gaing', [512], F32)
    cst = AR.alloc('cst', [8], F32)
    nsp8 = AR.alloc('nsp8', [4], F32)
    tmp4 = AR.alloc('tmp4', [4], F32)
    raw = AR.alloc('raw', [4, 516], F32)
    praw = AR.alloc('praw', [4, 516], F32)
    hcar = AR.alloc('hcar', [4], F32)
    kms = AR.alloc('kms', [4, 32], F32)
    xl2 = [AR.alloc('xl%d' % i, [512], F32) for i in range(2)]
    xlb2 = [AR.alloc('xlb%d' % i, [512], BF16) for i in range(2)]
    r_t = AR.alloc('r_t', [512], F32)
    i_t = AR.alloc('i_t', [512], F32)
    a_t = AR.alloc('a_t', [512], F32)
    q_t = AR.alloc('q_t', [512], F32)
    u_t = AR.alloc('u_t', [512], F32)
    yb = AR.alloc('yb', [4, 512], F32)
    sg = AR.alloc('sg', [4, 512], F32)
    scx = i_t
    sqb = AR.alloc('sqb', [2, 512], BF16)
    rt = AR.alloc('rt', [512], F32)
    rinv = AR.alloc('rinv', [512], F32)
    qo = AR.alloc('qo', [2, 512], BF16)
    ko = AR.alloc('ko', [2, 512], BF16)
    vo = AR.alloc('vo', [2, 512], BF16)
    go = AR.alloc('go', [2, 512], F32)
    zo = AR.alloc('zo', [2, 512], BF16)

    P.dma('sp', ident[:, :], dram(G.ident, 'c_ident'))
    P.dma('sp', pp[:, :], dram(G.pp[l], 'c_pp'))
    P.dma('sp', gaing[:, :], dram(bass.AP(G.bcp.tensor, l * (512 + 4 * D), [[0, 128], [1, 512]]), 'c_bcp'))
    stg = [yb.at(i)[:, i, :] for i in range(4)] + [sg.at(i)[:, i, :] for i in range(4)]
    lc = LoadCast(P, stg)
    for c in range(4):
        lc.go(bda[:, c, :], dram(G.bd_a[l, c], 'c_bda'), 128)
        lc.go(bdx[:, c, :], dram(G.bd_x[l, c], 'c_bdx'), 128)
    P.memset('pool', ones[:, :], 1.0)
    P.memset('pool', cst[:, 0:1], RMS_EPS)
    P.memset('pool', cst[:, 1:2], 1.0)
    P.memset('pool', raw[:, :, :], 0.0)
    P.memset('pool', praw[:, :, :], 0.0)
    P.memset('pool', hcar[:, :], 0.0)
    P.memset('pool', kms[:, :, :], 0.0)
    P.act(tmp4[:, :], pp[:, 28:32], AF.Exp, scale=-1.0)
    P.act(tmp4[:, :], tmp4[:, :], AF.Ln, bias=cst[:, 1:2])
    P.ts('dve', nsp8[:, :], tmp4[:, :], -8.0, None, ALU.mult)
    w_in_l = G.w_in[l].rearrange('(kc p) c -> p kc c', p=128)
    for s_ in (4, 0, 1, 2, 3, 5, 8, 7, 6, 9):
        for kc in range(8):
            lc.go(w_in.at(s_)[:, kc, s_ * 512:(s_ + 1) * 512],
                  dram(w_in_l[:, kc, s_ * 512:(s_ + 1) * 512], 'c_w_in'), 512)

    x_t = xin.rearrange('(t j p) d -> t p j d', j=4, p=128)

    def load_x(t):
        for j in range(4):
            P.dma('sp', xs.at(j)[:, j, :], dram(x_t[t][:, j, :], xin_name, t))

    bank_rr = [0]

    def nbank():
        b = PB[2 + bank_rr[0] % 6]
        bank_rr[0] += 1
        return b

    def fm(col0):
        b = nbank()
        s_ = col0 // 512
        for kc in range(8):
            P.mm(b[:, :], w_in.at(s_)[:, kc, col0:col0 + 128], xT[:, kc, :],
                 start=(kc == 0), stop=(kc == 7), inc=(kc == 7))
        return b

    def tm(col0, j):
        b = nbank()
        s_ = col0 // 512
        for kc in range(8):
            P.mm(b[:, :], xT[:, kc, j * 128:(j + 1) * 128], w_in.at(s_)[:, kc, col0:col0 + 512],
                 start=(kc == 0), stop=(kc == 7), inc=(kc == 7))
        return b

    def do_transposes(t):
        for kc in range(8):
            b = PB[kc % 2]
            for j in range(4):
                P.tr(b[:, j * 128:(j + 1) * 128], xs.at(j)[:, j, kc * 128:(kc + 1) * 128], ident[:, :], inc=(j == 3))
            if kc % 2 == 0:
                P.copy('act', xT[:, kc, :], b[:, :])
            else:
                P.copy('dve', xT[:, kc, :], b[:, :])
        if t + 1 < NT:
            load_x(t + 1)

    load_x(0)
    do_transposes(0)
    for t in range(NT):
        tok0 = t * 512
        def do_q(c):
            b = fm(0 + c * 128)
            P.ts('dve', qo.at(c % 2)[:, c % 2, :], b[:, :], 0.125, None, ALU.mult)
            P.dma('sp', dram(G.qT_d[c * 128:(c + 1) * 128, tok0:tok0 + 512], 'qT_d', t), qo.at(c % 2)[:, c % 2, :])

        def do_k(c):
            b = fm(512 + c * 128)
            for hb in range(2):
                P.act(ko.at(c % 2)[:, c % 2, hb * 256:(hb + 1) * 256], b[:, hb * 256:(hb + 1) * 256], AF.Copy,
                      accum=kms[:, c, 2 * t + hb:2 * t + hb + 1])
            P.dma('sp', dram(G.kT_d[c * 128:(c + 1) * 128, tok0:tok0 + 512], 'kT_d', t), ko.at(c % 2)[:, c % 2, :])

        def do_v(j):
            b = tm(1024, j)
            P.copy('dve', vo.at(j % 2)[:, j % 2, :], b[:, :])
            P.dma('sp', dram(G.V_d[tok0 + j * 128:tok0 + (j + 1) * 128, :], 'V_d', t), vo.at(j % 2)[:, j % 2, :])

        def do_g(j):
            b = tm(1536, j)
            P.act(go.at(j % 2)[:, j % 2, :], b[:, :], AF.Silu)
            P.tt('pool', go.at(j % 2)[:, j % 2, :], go.at(j % 2)[:, j % 2, :], gaing[:, :], ALU.mult)
            P.dma('sp', dram(G.gatt_d[tok0 + j * 128:tok0 + (j + 1) * 128, :], 'gatt_d', t), go.at(j % 2)[:, j % 2, :])

        def lru_front(c):
            xl_c = xl2[c % 2]
            b = fm(2048 + c * 128)
            P.copy('dve', raw.at(c)[:, c, 3:515], b[:, :])
            P.ts('dve', xl_c[:, :], raw.at(c)[:, c, 3:515], pp[:, c * 4 + 3:c * 4 + 4], pp[:, 16 + c:17 + c], ALU.mult, ALU.add)
            for k in range(3):
                P.stt(xl_c[:, :], raw.at(c)[:, c, k:k + 512], pp[:, c * 4 + k:c * 4 + k + 1], xl_c[:, :], ALU.mult, ALU.add)
            P.copy('pool', raw.at(c)[:, c, 0:3], raw.at(c)[:, c, 512:515])
            P.copy('pool', xlb2[c % 2][:, :], xl_c[:, :])

        def lru_chain(c):
            xl_c = xl2[c % 2]
            xlb_c = xlb2[c % 2]
            bg = nbank()
            P.mm(bg[:, :], bda[:, c, :], xlb_c[:, :])
            P.act(r_t[:, :], bg[:, :], AF.Sigmoid, bias=pp[:, 20 + c:21 + c])
            bg2 = nbank()
            P.mm(bg2[:, :], bdx[:, c, :], xlb_c[:, :])
            P.act(i_t[:, :], bg2[:, :], AF.Sigmoid, bias=pp[:, 24 + c:25 + c])
            P.act(a_t[:, :], r_t[:, :], AF.Exp, scale=nsp8[:, c:c + 1])
            P.act(q_t[:, :], a_t[:, :], AF.Square)
            P.act(q_t[:, :], q_t[:, :], AF.Sqrt, bias=cst[:, 1:2], scale=-1.0)
            P.tt('pool', u_t[:, :], i_t[:, :], xl_c[:, :], ALU.mult)
            P.tt('dve', u_t[:, :], u_t[:, :], q_t[:, :], ALU.mult)
            P.scan(yb.at(c)[:, c, :], a_t[:, :], u_t[:, :], hcar.at(c)[:, c:c + 1])
            P.copy('pool', hcar.at(c)[:, c:c + 1], yb.at(c)[:, c, 511:512])

        for c in range(4):
            lru_front(c)
            do_q(c)
            do_k(c)
            do_v(c)
            if c >= 1:
                lru_chain(c - 1)
            do_g(c)
        lru_chain(3)

        def norm1a(gate_col0):
            for c in range(4):
                b = fm(gate_col0 + c * 128)
                P.act(sg.at(c)[:, c, :], b[:, :], AF.Silu)

        def norm1(gate_col0):
            norm1a(gate_col0)
            norm1b()

        def norm1b():
            bs = nbank()
            for c in range(4):
                P.tt('pool', sqb.at(c % 2)[:, c % 2, :], yb.at(c)[:, c, :], yb.at(c)[:, c, :], ALU.mult)
                P.mm(bs[:, :], ones[:, :], sqb.at(c % 2)[:, c % 2, :], start=(c == 0), stop=(c == 3), inc=True)
            P.act(rt[:, :], bs[:, :], AF.Sqrt, bias=cst[:, 0:1], scale=1.0 / 512.0)
            P.recip(rinv[:, :], rt[:, :])

        def norm2(grp, gain_col):
            for c in range(4):
                P.stt(yb.at(c)[:, c, :], yb.at(c)[:, c, :], pp[:, gain_col + c:gain_col + c + 1], rinv[:, :], ALU.mult, ALU.mult)
                P.tt('pool', zo.at(c % 2)[:, c % 2, :], yb.at(c)[:, c, :], sg.at(c)[:, c, :], ALU.mult)
                r0 = (grp - 1) * 512 + c * 128
                P.dma('sp', dram(G.zT_d[r0:r0 + 128, tok0:tok0 + 512], 'zT_d', (grp, t)), zo.at(c % 2)[:, c % 2, :])

        def sc_front(c):
            bx = fm(4096 + c * 128)
            P.copy('dve', scx[:, :], bx[:, :])
            bc_ = fm(3584 + c * 128)
            P.tt('dve', praw.at(c)[:, c, 2:514], bc_[:, :], scx[:, :], ALU.mult)
            xc = xl2[c % 2]
            P.ts('dve', xc[:, :], praw.at(c)[:, c, 2:514], pp[:, 32 + c * 3 + 2:32 + c * 3 + 3], None, ALU.mult)
            for k in range(2):
                P.stt(xc[:, :], praw.at(c)[:, c, k:k + 512], pp[:, 32 + c * 3 + k:32 + c * 3 + k + 1], xc[:, :], ALU.mult, ALU.add)
            P.copy('pool', praw.at(c)[:, c, 0:2], praw.at(c)[:, c, 512:514])

        def sc_back(c):
            bb = fm(3072 + c * 128)
            P.tt('dve', yb.at(c)[:, c, :], bb[:, :], xl2[c % 2][:, :], ALU.mult)

        norm1(2560)
        sc_front(0)
        norm2(1, 44)
        sc_back(0)
        for c in range(1, 4):
            sc_front(c)
            sc_back(c)
        norm1a(4608)
        if t + 1 < NT:
            do_transposes(t + 1)
        norm1b()
        norm2(2, 48)
    for c in range(4):
        P.dma('sp', dram(G.km_d[c * 128:(c + 1) * 128, :], 'km_d'), kms[:, c, :])


def group_norm_out(G, l, t, grp, fm, gate_col0, yb, sg, sqb, rt, rinv, zo, cst, ones, pp, gain_col, nbank):
    P = G.P
    tok0 = t * 512
    for c in range(4):
        b = fm(gate_col0 + c * 128)
        P.act(sg.at(c)[:, c, :], b[:, :], AF.Silu)
    bs = nbank()
    for c in range(4):
        P.act(sqb.at(c % 2)[:, c % 2, :], yb.at(c)[:, c, :], AF.Square)
        P.mm(bs[:, :], ones[:, :], sqb.at(c % 2)[:, c % 2, :], start=(c == 0), stop=(c == 3), inc=True)
    P.act(rt[:, :], bs[:, :], AF.Sqrt, bias=cst[:, 0:1], scale=1.0 / 512.0)
    P.recip(rinv[:, :], rt[:, :])
    for c in range(4):
        P.stt(yb.at(c)[:, c, :], yb.at(c)[:, c, :], pp[:, gain_col + c:gain_col + c + 1], rinv[:, :], ALU.mult, ALU.mult)
        P.tt('pool', zo.at(c % 2)[:, c % 2, :], yb.at(c)[:, c, :], sg.at(c)[:, c, :], ALU.mult)
        r0 = (grp - 1) * 512 + c * 128
        P.dma('sp', dram(G.zT_d[r0:r0 + 128, tok0:tok0 + 512], 'zT_d', (grp, t)), zo.at(c % 2)[:, c % 2, :])


def stage_B(G, l):
    P, AR, PB = G.P, G.AR, G.PB
    ident = AR.alloc('identB', [128], F32)
    identb = AR.alloc('identb', [128], BF16)
    trib = AR.alloc('trib', [2, 256], BF16)
    vb1 = AR.alloc('vb1', [2048], F32)
    vb2 = AR.alloc('vb2', [2048], F32)
    kmf = AR.alloc('kmf', [4, 32], F32)
    kmb = AR.alloc('kmb', [8, 32], BF16)
    stg = AR.alloc('stgB', [2048], F32)
    qa = [AR.alloc('qa%d' % i, [S], BF16) for i in range(2)]
    ka = [AR.alloc('ka%d' % i, [S], BF16) for i in range(2)]
    va = [AR.alloc('va%d' % i, [64, 65], BF16) for i in range(2)]
    gm = AR.alloc('gm', [16, 32], F32)
    gm2 = AR.alloc('gm2', [16, 32], F32)
    top8 = AR.alloc('top8', [16, 8], F32)
    mbp = AR.alloc('mbp', [16, 96], F32)
    pT = [AR.alloc('pT%d' % i, [512], BF16) for i in range(4)]
    yo = AR.alloc('yo', [2, 4, 64], F32)
    rec = AR.alloc('rec', [2, 4], F32)

    P.dma('sp', ident[:, :], dram(G.ident, 'c_ident'))
    P.copy('dve', identb[:, :], ident[:, :])
    for r in range(2):
        P.dma('sp', stg[:, 0:256], dram(G.trib[r], 'c_trib'))
        P.copy('dve', trib[:, r, :], stg[:, 0:256])
    P.dma('sp', vb1[:, :], dram(bass.AP(G.vb.tensor, 0, [[0, 128], [1, 2048]]), 'c_vb'))
    P.dma('sp', vb2[:, :], dram(bass.AP(G.vb.tensor, 2048, [[0, 128], [1, 2048]]), 'c_vb'))
    for c in range(4):
        P.dma('sp', kmf[:, c, :], dram(G.km_d[c * 128:(c + 1) * 128, :], 'km_d'))
    P.memset('pool', mbp[:, :, :], 0.0)
    for i in range(2):
        P.memset('pool', va[i].at('one')[:, :, 64:65], 1.0)
        for c4 in range(4):
            sv = V(stg.ap[64:96, :], stg[:, :].key)
            P.dma('sp', sv, dram(G.E[:, c4 * 2048:(c4 + 1) * 2048], 'c_E'))
            P.copy('dve', V(ka[i].ap[64:96, c4 * 2048:(c4 + 1) * 2048], (ka[i].name, 'E')), sv)
    for h in range(8):
        par = h % 2
        P.copy('dve', V(kmb.ap[0:64, h, :], (kmb.name, h)), V(kmf.ap[64 * par:64 * par + 64, h // 2, :], (kmf.name, None)))

    v_r = G.V_d.rearrange('(t p) c -> p t c', p=128)
    y_r = G.yatt_d.rearrange('(t j p) c -> t p j c', j=4, p=128)

    def load_head(h):
        i = h % 2
        P.dma('sp', V(qa[i].ap[0:64, :], (qa[i].name, 'q')), dram(G.qT_d[64 * h:64 * h + 64, :], 'qT_d'))
        P.dma('sp', V(ka[i].ap[0:64, :], (ka[i].name, 'k')), dram(G.kT_d[64 * h:64 * h + 64, :], 'kT_d'))
        for g in range(4):
            P.dma('sp', V(va[i].ap[:, g * 16:(g + 1) * 16, 0:64], (va[i].name, 'v')),
                  dram(v_r[:, g * 16:(g + 1) * 16, 64 * h:64 * h + 64], 'V_d'))

    def gate_A(h, g16):
        i = h % 2
        if True:
            for s16 in range(16):
                sub = g16 * 16 + s16
                P.mm(PB[6][:, s16 * 32:(s16 + 1) * 32],
                     V(qa[i].ap[0:64, sub * 128:(sub + 1) * 128], (qa[i].name, 'q')),
                     V(kmb.ap[0:64, h, :], (kmb.name, h)), start=True, stop=True, inc=(s16 == 15))
            gmf = V(gm.ap[:, :, :].rearrange('p a b -> p (a b)'), gm[:, :, :].key)
            gm2f = V(gm2.ap[:, :, :].rearrange('p a b -> p (a b)'), gm2[:, :, :].key)
            P.tt('dve', gmf, PB[6][:, :], vb1[:, g16 * 512:(g16 + 1) * 512], ALU.add)
            P.tt('dve', gm2f, PB[6][:, :], vb2[:, g16 * 512:(g16 + 1) * 512], ALU.add)
            for s16 in range(16):
                P.max8(top8.at(s16)[:, s16, :], gm[:, s16, :])
            for s16 in range(16):
                P.ts('dve', mbp.at(s16)[:, s16, 64:96], gm2[:, s16, :], top8.at(s16)[:, s16, 2:3], -BIG, ALU.is_lt, ALU.mult)
    def gate_B(h, g16, dve_only=False):
        i = h % 2
        if True:
            for g4 in range(4):
                for s4 in range(4):
                    s16 = g4 * 4 + s4
                    P.tr(V(PB[7].ap[0:96, s4 * 128:(s4 + 1) * 128], PB[7][:, :].key), mbp.at(s16)[:, s16, 0:96], ident[:, :], inc=(s4 == 3))
                sub0 = g16 * 16 + g4 * 4
                eng = 'act' if (g4 % 2 == 0 and not dve_only) else 'dve'
                P.copy(eng, V(qa[i].ap[64:96, sub0 * 128:sub0 * 128 + 512], (qa[i].name, 'm')),
                       V(PB[7].ap[64:96, :], PB[7][:, :].key))

    def gate_phase(h):
        for g16 in range(4):
            gate_A(h, g16)
            gate_B(h, g16)

    def main_phase(h, hooks=None):
        i = h % 2
        qkeys = [(qa[i].name, 'q'), (qa[i].name, 'm')]
        kkeys = [(ka[i].name, 'k'), (ka[i].name, 'E')]
        vkeys = [(va[i].name, 'v'), (va[i].name, 'one')]
        units = []
        for qt in range(NT):
            for kt in range(4 * (qt + 1)):
                units.append((qt, kt))
        N = len(units)
        DEPTH = 3

        def emit_S(n):
            qt, kt = units[n]
            i_d = kt - 4 * qt
            c0 = 128 * i_d if i_d > 0 else 0
            sb = PB[2 + n % 4]
            P.mm(sb[:, c0:512], V(ka[i].ap[0:96, kt * 128:(kt + 1) * 128], kkeys),
                 V(qa[i].ap[0:96, qt * 512 + c0:(qt + 1) * 512], qkeys), start=True, stop=(i_d < 0), inc=(i_d < 0))
            if i_d >= 0:
                m, r = i_d // 2, i_d % 2
                lo, hi = max(c0, 256 * m), 256 * m + 256
                P.mm(sb[:, lo:hi], identb[:, :], trib[:, r, lo - 256 * m:256], start=False, stop=True, inc=True)
            P.act(pT[n % 4][:, c0:512], sb[:, c0:512], AF.Exp)

        def emit_PV(n):
            qt, kt = units[n]
            i_d = kt - 4 * qt
            c0 = 128 * i_d if i_d > 0 else 0
            acc = PB[qt % 2]
            last_kt = (kt == 4 * (qt + 1) - 1)
            for j in range(4):
                if 128 * j < c0:
                    continue
                P.mm(acc[:, j * 128:j * 128 + 65], pT[n % 4][:, j * 128:(j + 1) * 128],
                     V(va[i].ap[:, kt, 0:65], vkeys), start=(kt == 0 and j == 0), stop=(last_kt and j == 3),
                     inc=(j == 3))
            if last_kt:
                sl = qt % 2
                accv = acc.ap[:, :].rearrange('p (j c) -> p j c', j=4)
                P.recip(V(rec.ap[:, sl, :].unsqueeze(2), (rec.name, sl)), V(accv[:, :, 64:65], acc[:, :].key))
                P.tt('dve', yo.at(sl)[:, sl, :, :], V(accv[:, :, 0:64], acc[:, :].key),
                     V(rec.ap[:, sl, :].unsqueeze(2).broadcast_to([128, 4, 64]), (rec.name, sl)), ALU.mult)
                P.dma('sp', dram(y_r[qt][:, :, 64 * h:64 * h + 64], 'yatt_d', (h, qt)), yo.at(sl)[:, sl, :, :])

        for n in range(N + DEPTH):
            if hooks and n in hooks:
                hooks[n]()
            if n < N:
                emit_S(n)
            if n - DEPTH >= 0:
                emit_PV(n - DEPTH)

    load_head(0)
    gate_phase(0)
    for h in range(NHEAD_RUN):
        hooks = None
        if h + 1 < NHEAD_RUN:
            load_head(h + 1)
            hooks = {}
            for g16 in range(4):
                hooks[30 + 120 * g16] = (lambda hh=h + 1, g=g16: gate_A(hh, g))
                hooks[95 + 120 * g16] = (lambda hh=h + 1, g=g16: gate_B(hh, g, dve_only=True))
        main_phase(h, hooks)


def alias(tt, shape, dt):
    ap = tt.ap
    nd = len(ap.shape)
    if nd == 3:
        ap = ap.rearrange('p a b -> p (a b)')
    elif nd == 4:
        ap = ap.rearrange('p a b c -> p (a b c)')
    if ap.dtype != dt:
        ap = ap.bitcast(dt)
    n = 1
    for s_ in shape:
        n *= s_
    ap = ap[:, 0:n]
    if len(shape) == 2:
        ap = ap.rearrange('p (a b) -> p a b', a=shape[0])
    elif len(shape) == 3:
        ap = ap.rearrange('p (a b c) -> p a b c', a=shape[0], b=shape[1])
    return TT(tt.name, ap)


def stage_C(G, l, xin, xin_name, xout, xout_name):
    P, AR, PB = G.P, G.AR, G.PB
    w_out = AR.alloc('w_out', [12, 1024], BF16)
    wq = AR.alloc('wq', [8, 1024], BF16)
    wo = AR.alloc('wo', [8, 1024], BF16)
    KmT = AR.alloc('KmT', [8, 256], BF16)
    Vm = AR.alloc('Vm', [2, 4, 258], BF16)
    ident = AR.alloc('identC', [128], F32)
    gbc = AR.alloc('gbc', [4, 1024], F32)
    cst = AR.alloc('cstC', [8], F32)
    xres = [AR.alloc('xres%d' % i, [4, 1024], F32) for i in range(2)]
    bufAB = AR.alloc('bufAB', [4, 1024], F32)
    zT = AR.alloc('zT', [12, 512], BF16)
    x1T = AR.alloc('x1T', [8, 512], BF16)
    qT = AR.alloc('qTc', [8, 512], BF16)
    pT = [[AR.alloc('pTc%d_%d' % (i, m), [512], BF16) for m in range(2)] for i in range(2)]
    ss = AR.alloc('ss', [4], F32)
    rt4 = AR.alloc('rt4', [4], F32)
    r4 = AR.alloc('r4', [4], F32)
    st = AR.alloc('st', [4, 2, 6], F32)
    mv = AR.alloc('mv', [4, 2], F32)
    sd = AR.alloc('sd', [4], F32)
    rstd = AR.alloc('rstd', [4], F32)
    nb_ = AR.alloc('nb', [4], F32)
    rc = AR.alloc('rc', [8], F32)
    oT = alias(x1T, [8, 512], BF16)
    junk = alias(qT, [512], F32)
    obuf = AR.alloc('obuf', [4, 1024], F32)
    wk_v = alias(bufAB, [8, 1024], BF16)
    wv_v = alias(xres[0], [8, 1024], BF16)
    memT = alias(x1T, [8, 256], BF16)

    P.dma('sp', ident[:, :], dram(G.ident, 'c_ident'))
    for i in range(4):
        P.dma('sp', gbc[:, i, :], dram(bass.AP(G.bcp.tensor, l * (512 + 4 * D) + 512 + i * D, [[0, 128], [1, D]]), 'c_bcp'))
    P.memset('pool', cst[:, 0:1], RMS_EPS)
    P.memset('pool', cst[:, 1:2], LN_EPS)
    P.memset('pool', Vm[:, :, :, 256:257], 1.0)
    stg = [V(xres[1].ap[:, j, h2 * 512:(h2 + 1) * 512], (xres[1].name, (j, h2))) for j in range(4) for h2 in range(2)]
    lc = LoadCast(P, stg)

    def load_w(dst, src2d, nk):
        src = src2d.rearrange('(kc p) c -> p kc c', p=128)
        for kc in range(nk):
            for h2 in range(2):
                lc.go(dst[:, kc, h2 * 512:(h2 + 1) * 512], dram(src[:, kc, h2 * 512:(h2 + 1) * 512], 'c_w'), 512)

    mt = G.memT.rearrange('(kc p) m -> p kc m', p=128)
    for kc in range(8):
        lc.go(memT[:, kc, :], dram(mt[:, kc, :], 'c_mem'), 256)
    load_w(wk_v, G.wk[l], 8)
    load_w(wv_v, G.wv[l], 8)
    rr = [0]

    def nbank():
        b = PB[2 + rr[0] % 6]
        rr[0] += 1
        return b

    ev = [0]

    def evac(out, in_, scale=None):
        ev[0] += 1
        if scale is not None:
            P.act(out, in_, AF.Copy, scale=scale)
        else:
            P.copy('act', out, in_)

    for oc in range(8):
        b = nbank()
        for kc in range(8):
            P.mm(b[:, 0:256], wk_v[:, kc, oc * 128:(oc + 1) * 128], memT[:, kc, :], start=(kc == 0), stop=(kc == 7), inc=(kc == 7))
        evac(KmT[:, oc, :], b[:, 0:256])
    for mh in range(2):
        for half in range(2):
            b = nbank()
            for kc in range(8):
                P.mm(b[:, :], memT[:, kc, mh * 128:(mh + 1) * 128], wv_v[:, kc, half * 512:(half + 1) * 512],
                     start=(kc == 0), stop=(kc == 7), inc=(kc == 7))
            evac(Vm[:, mh, 2 * half:2 * half + 2, 0:256], V(b.ap[:, :].rearrange('p (a b) -> p a b', a=2), b[:, :].key))
    load_w(w_out, G.w_out[l], 12)
    load_w(wq, G.wq[l], 8)
    load_w(wo, G.wo[l], 8)

    x_t = xin.rearrange('(t j p) d -> t p j d', j=4, p=128)
    o_t = xout.rearrange('(t j p) d -> t p j d', j=4, p=128)
    ya_t = G.yatt_d.rearrange('(t j p) c -> t p j c', j=4, p=128)
    ga_t = G.gatt_d.rearrange('(t j p) c -> t p j c', j=4, p=128)
    zT_r = G.zT_d.rearrange('(kc p) t -> p kc t', p=128)

    def ln_p1(xr, j):
        for h2 in range(2):
            P.bn_stats(st.at(j)[:, j, h2, :], xr.at(j)[:, j, h2 * 512:(h2 + 1) * 512])
        P.bn_aggr(mv.at(j)[:, j, :], V(st.ap[:, j, :, :].rearrange('p a b -> p (a b)'), (st.name, j)))
        P.act(sd.at(j)[:, j:j + 1], mv.at(j)[:, j, 1:2], AF.Sqrt, bias=cst[:, 1:2])

    def ln_p2(xr, j):
        P.recip(rstd.at(j)[:, j:j + 1], sd.at(j)[:, j:j + 1])
        P.ts('dve', nb_.at(j)[:, j:j + 1], mv.at(j)[:, j, 0:1], -1.0, rstd.at(j)[:, j:j + 1], ALU.mult, ALU.mult)
        P.act(xr.at(j)[:, j, :], xr.at(j)[:, j, :], AF.Identity, bias=nb_.at(j)[:, j:j + 1], scale=rstd.at(j)[:, j:j + 1])

    def ln_p3(xr, j, gi):
        P.tt('dve', xr.at(j)[:, j, :], xr.at(j)[:, j, :], gbc[:, gi, :], ALU.mult)
        P.tt('pool', xr.at(j)[:, j, 0:512], xr.at(j)[:, j, 0:512], gbc[:, gi + 1, 0:512], ALU.add)
        P.tt('dve', xr.at(j)[:, j, 512:1024], xr.at(j)[:, j, 512:1024], gbc[:, gi + 1, 512:1024], ALU.add)

    def ln_pipeline(xr, gi, mm_fn, after_fn, mid_fn=None, mid_early=False):
        if mid_early:
            sched = [('1', 0), ('1', 1), ('M', 0), ('2', 0), ('1', 2), ('2', 1), ('3', 0), ('1', 3), ('A', 0), ('2', 2),
                     ('3', 1), ('A', 1), ('2', 3), ('3', 2), ('A', 2), ('3', 3), ('A', 3)]
        else:
            sched = [('1', 0), ('1', 1), ('2', 0), ('1', 2), ('2', 1), ('3', 0), ('1', 3), ('M', 0), ('A', 0), ('2', 2),
                     ('3', 1), ('A', 1), ('2', 3), ('3', 2), ('A', 2), ('3', 3), ('A', 3)]
        for ph, j in sched:
            if ph == '1':
                mm_fn(j)
                ln_p1(xr, j)
            elif ph == '2':
                ln_p2(xr, j)
            elif ph == '3':
                ln_p3(xr, j, gi)
            elif ph == 'M':
                if mid_fn is not None:
                    mid_fn()
            else:
                after_fn(j)

    tr_rr = [0]

    def transposes_j(src_fn, dst, j, nkc):
        for g in range(nkc // 4):
            b = PB[tr_rr[0] % 2]
            tr_rr[0] += 1
            for k4 in range(4):
                P.tr(b[:, k4 * 128:(k4 + 1) * 128], src_fn(j, g * 4 + k4), ident[:, :], inc=(k4 == 3))
            evac(V(dst.ap[:, g * 4:(g + 1) * 4, j * 128:(j + 1) * 128], dst[:, :, :].key),
                 V(b.ap[:, :].rearrange('p (a b) -> p a b', a=4), b[:, :].key))

    def transposes(src_fn, dst, nkc):
        for kc in range(nkc):
            b = PB[kc % 2]
            for j in range(4):
                P.tr(b[:, j * 128:(j + 1) * 128], src_fn(j, kc), ident[:, :], inc=(j == 3))
            evac(dst[:, kc, :], b[:, :])

    def load_tile(T):
        xr = xres[T % 2]
        for j in range(4):
            P.dma('sp', xr.at(j)[:, j, :], dram(x_t[T][:, j, :], xin_name, T))
        P.dma('sp', bufAB[:, :, 0:512], dram(ya_t[T], 'yatt_d'))
        P.dma('sp', bufAB[:, :, 512:1024], dram(ga_t[T], 'gatt_d'))

    def load_tile_b(T):
        P.dma('sp', zT[:, 4:12, :], dram(zT_r[:, :, T * 512:(T + 1) * 512], 'zT_d'))

    P.memset('pool', xres[1][:, 0, 0:1], 0.0)
    def att_norm():
        for j in range(4):
            P.act(junk[:, :], bufAB[:, j, 0:512], AF.Square, accum=ss.at(j)[:, j:j + 1])
        P.act(rt4[:, :], ss[:, :], AF.Sqrt, bias=cst[:, 0:1], scale=1.0 / 512.0)
        P.recip(r4[:, :], rt4[:, :])
        for j in range(4):
            P.stt(bufAB[:, j, 0:512], bufAB[:, j, 0:512], r4[:, j:j + 1], bufAB[:, j, 512:1024], ALU.mult, ALU.mult)

    def z_att_tr():
        transposes(lambda j, kc: bufAB[:, j, kc * 128:(kc + 1) * 128], zT, 4)

    load_tile(0)
    load_tile_b(0)
    att_norm()
    z_att_tr()
    for T in range(NT):
        xr = xres[T % 2]
        if T + 1 < NT:
            load_tile(T + 1)
        def mm_wout(j, xr=xr):
            for h2 in range(2):
                b = nbank()
                for kc in range(12):
                    P.mm(b[:, :], zT[:, kc, j * 128:(j + 1) * 128], w_out[:, kc, h2 * 512:(h2 + 1) * 512],
                         start=(kc == 0), stop=(kc == 11), inc=(kc == 11))
                P.stt(xr.at(j)[:, j, h2 * 512:(h2 + 1) * 512], xr.at(j)[:, j, h2 * 512:(h2 + 1) * 512], ALPHA, b[:, :], ALU.mult, ALU.add)

        ln_pipeline(xr, 0, mm_wout,
                    lambda j, xr=xr: transposes_j(lambda jj, kc: xr.at(jj)[:, jj, kc * 128:(kc + 1) * 128], x1T, j, 8),
                    mid_fn=(lambda T=T: load_tile_b(T + 1)) if T + 1 < NT else None)
        for oc in range(8):
            b = nbank()
            for kc in range(8):
                P.mm(b[:, :], wq[:, kc, oc * 128:(oc + 1) * 128], x1T[:, kc, :], start=(kc == 0), stop=(kc == 7), inc=(kc == 7))
            evac(qT[:, oc, :], b[:, :], scale=1.0 / 16.0)

        def emit_S(hd):
            pb_ = pT[hd % 2]
            for mh in range(2):
                b = nbank()
                for cc in range(2):
                    P.mm(b[:, :], KmT[:, 2 * hd + cc, mh * 128:(mh + 1) * 128], qT[:, 2 * hd + cc, :],
                         start=(cc == 0), stop=(cc == 1), inc=(cc == 1))
                P.act(pb_[mh][:, :], b[:, :], AF.Exp)

        def emit_PV(hd):
            pb_ = pT[hd % 2]
            for j in range(4):
                b = nbank()
                for mh in range(2):
                    P.mm(b[:, 0:257], pb_[mh][:, j * 128:(j + 1) * 128], Vm[:, mh, hd, 0:257],
                         start=(mh == 0), stop=(mh == 1), inc=(mh == 1))
                k8 = (hd * 4 + j) % 8
                P.recip(rc.at(k8)[:, k8:k8 + 1], b[:, 256:257])
                P.ts('dve', obuf[:, j, hd * 256:(hd + 1) * 256], b[:, 0:256], rc.at(k8)[:, k8:k8 + 1], None, ALU.mult)

        emit_S(0)
        for hd in range(4):
            if hd + 1 < 4:
                emit_S(hd + 1)
            emit_PV(hd)
        transposes(lambda j, kc: obuf[:, j, kc * 128:(kc + 1) * 128], oT, 8)
        if T + 1 < NT:
            att_norm()
        def mm_wo(j, xr=xr):
            for h2 in range(2):
                b = nbank()
                for kc in range(8):
                    P.mm(b[:, :], oT[:, kc, j * 128:(j + 1) * 128], wo[:, kc, h2 * 512:(h2 + 1) * 512],
                         start=(kc == 0), stop=(kc == 7), inc=(kc == 7))
                P.stt(xr.at(j)[:, j, h2 * 512:(h2 + 1) * 512], xr.at(j)[:, j, h2 * 512:(h2 + 1) * 512], ALPHA, b[:, :], ALU.mult, ALU.add)

        ln_pipeline(xr, 2, mm_wo,
                    lambda j, xr=xr, T=T: P.dma('sp', dram(o_t[T][:, j, :], xout_name, T), xr.at(j)[:, j, :]),
                    mid_fn=z_att_tr if T + 1 < NT else None, mid_early=True)


def host_constants():
    ident = np.eye(128, dtype=np.float32)
    E = np.zeros((32, S), np.float32)
    for j in range(32):
        E[j, j * 256:(j + 1) * 256] = 1.0
    trib = np.zeros((2, 128, 256), np.float32)
    kk = np.arange(128)[:, None]
    qq = np.arange(256)[None, :]
    trib[0] = np.where(qq >= kk, 0.0, -BIG)
    trib[1] = np.where(qq >= kk + 128, 0.0, -BIG)
    vb = np.zeros((2, 1, 64, 32), np.float32)
    for sub in range(64):
        own = sub // 2
        for blk in range(32):
            if blk < own:
                vb[0, 0, sub, blk] = 0.0
                vb[1, 0, sub, blk] = 0.0
            elif blk == own:
                vb[0, 0, sub, blk] = -BIG
                vb[1, 0, sub, blk] = BIG
            else:
                vb[0, 0, sub, blk] = -BIG
                vb[1, 0, sub, blk] = -2 * BIG
    return ident, E, trib, vb.reshape(2, 1, 64 * 32)


def host_layout(inputs):
    f = lambda n: np.ascontiguousarray(np.asarray(inputs[n], dtype=np.float32))
    lw_a, lw_x = f('lru_w_a'), f('lru_w_x')
    bd_a = np.zeros((L, 4, 128, 128), np.float32)
    bd_x = np.zeros((L, 4, 128, 128), np.float32)
    for l in range(L):
        for c in range(4):
            for hh in range(2):
                bd_a[l, c, hh * 64:(hh + 1) * 64, hh * 64:(hh + 1) * 64] = lw_a[l, 2 * c + hh]
                bd_x[l, c, hh * 64:(hh + 1) * 64, hh * 64:(hh + 1) * 64] = lw_x[l, 2 * c + hh]
    pp = np.zeros((L, 128, NPP), np.float32)
    lcw, lcb, b_a, b_x, lam = f('lru_conv_w'), f('lru_conv_b'), f('lru_b_a'), f('lru_b_x'), f('lru_lambda')
    scw, gg = f('sc_conv_w'), f('group_gain')
    for l in range(L):
        for c in range(4):
            sl = slice(c * 128, (c + 1) * 128)
            for k in range(4):
                pp[l, :, c * 4 + k] = lcw[l, k, sl]
            pp[l, :, 16 + c] = lcb[l, sl]
            pp[l, :, 20 + c] = b_a[l, sl]
            pp[l, :, 24 + c] = b_x[l, sl]
            pp[l, :, 28 + c] = lam[l, sl]
            for k in range(3):
                pp[l, :, 32 + c * 3 + k] = scw[l, k, sl]
            pp[l, :, 44 + c] = gg[l, 1, sl]
            pp[l, :, 48 + c] = gg[l, 2, sl]
    bcp = np.concatenate([gg[:, 0, :], f('ln1_g'), f('ln1_b'), f('ln2_g'), f('ln2_b')], axis=1).reshape(L, 1, 512 + 4 * D)
    ident, E, trib, vb = host_constants()
    shared = dict(w_in=f('w_in'), w_out=f('w_out'), wq=f('xq_w'), wk=f('xk_w'), wv=f('xv_w'), wo=f('xo_w'),
                  bd_a=bd_a, bd_x=bd_x, pp=pp, bcp=np.ascontiguousarray(bcp), ident=ident, E=E, trib=trib, vb=vb)
    x, mem = f('x'), f('mem')
    maps = []
    for b in range(x.shape[0]):
        m = dict(shared)
        m['x'] = np.ascontiguousarray(x[b])
        m['memT'] = np.ascontiguousarray(mem[b].T)
        maps.append(m)
    return maps


_NC_CACHE = {}


def kernel(**inputs):
    maps = host_layout(inputs)
    if 'nc' not in _NC_CACHE:
        _NC_CACHE['nc'] = build_program()
    nc = _NC_CACHE['nc']
    B = len(maps)
    in_maps = [maps[i % B] for i in range(8)]
    res = run_bass_kernel_spmd(nc, in_maps, core_ids=list(range(8)))
    out = np.stack([np.asarray(res.results[b]['out'], dtype=np.float32) for b in range(B)], axis=0)
    return out
```
